# Optimizing a Trainium2 kernel written in Bass

```python
import math
import jax
import jax.numpy as jnp
from jax import lax
import numpy as np


D_MODEL = 1024
BATCH = 8
SEQ = 4096
DEPTH = 2

EPS = 1e-6
NEG_INF = -1e30
FORCED_SCORE = 1e9
NSA_HEADS = 8
NSA_KV_GROUPS = 2
NSA_HEAD_DIM = 64
CMP_BLOCK = 32
CMP_STRIDE = 16
CMP_HIDDEN = 256
SEL_BLOCK = 64
SEL_TOP_N = 16
WINDOW = 512
NSA_Q_BLOCK = 64
MLA_HEADS = 8
MLA_Q_RANK = 256
MLA_KV_RANK = 128
MLA_NOPE_DIM = 64
MLA_ROPE_DIM = 32
MLA_V_DIM = 64
ROPE_THETA = 10000.0
ATTN_Q_BLOCK = 128
REL_BUCKETS = 32
REL_MAX_DIST = 128
CONV_WIDTH = 3
D_FF = 2816
N_EXPERTS = 8
TOP_K = 2
D_FF_EXPERT = 1408
MOE_ROW_BLOCK = 256
NSA_Q_W = NSA_HEADS * NSA_HEAD_DIM
NSA_KV_W = NSA_KV_GROUPS * NSA_HEAD_DIM
NSA_GATE_W = NSA_HEADS * 3
EVEN_IN_SIZES = (NSA_Q_W,) + (NSA_KV_W,) * 6 + (NSA_GATE_W, MLA_Q_RANK, MLA_KV_RANK, MLA_ROPE_DIM)
EVEN_IN_W = sum(EVEN_IN_SIZES)
MIX_OUT_W = NSA_HEADS * NSA_HEAD_DIM + MLA_HEADS * MLA_V_DIM

kernel_name = 'hybrid_nsa_mla_shortconv_moe'


def rms_norm(x, g):
    xf = x.astype(jnp.float32)
    y = xf * lax.rsqrt(jnp.mean(jnp.square(xf), axis=-1, keepdims=True) + EPS)
    return (y * g.astype(jnp.float32)).astype(x.dtype)


def masked_softmax(s, mask):
    p = jax.nn.softmax(jnp.where(mask, s, NEG_INF), axis=-1)
    return jnp.where(mask, p, 0.0)


def t5_bucket(dist):
    n = jnp.maximum(dist, 0)
    max_exact = REL_BUCKETS // 2
    nf = jnp.maximum(n, max_exact).astype(jnp.float32)
    large = max_exact + (jnp.log(nf / max_exact) / math.log(REL_MAX_DIST / max_exact)
                         * (REL_BUCKETS - max_exact)).astype(jnp.int32)
    return jnp.where(n < max_exact, n, jnp.minimum(large, REL_BUCKETS - 1))


def rope(x, pos):
    half = x.shape[-1] // 2
    inv_freq = ROPE_THETA ** (-jnp.arange(half, dtype=jnp.float32) / half)
    ang = pos.astype(jnp.float32)[:, None] * inv_freq[None, :]
    cos = jnp.cos(ang)[:, None, :]
    sin = jnp.sin(ang)[:, None, :]
    xf = x.astype(jnp.float32)
    x1, x2 = xf[..., :half], xf[..., half:]
    return jnp.concatenate([x1 * cos - x2 * sin, x1 * sin + x2 * cos], axis=-1).astype(x.dtype)


def compress(kv, pos_emb, w1, w2):
    B, S, G, dk = kv.shape
    nc = (S - CMP_BLOCK) // CMP_STRIDE + 1
    idx = jnp.arange(nc)[:, None] * CMP_STRIDE + jnp.arange(CMP_BLOCK)[None, :]
    blk = kv[:, idx] + pos_emb[None, None, :, None, :]
    blk = blk.transpose(0, 1, 3, 2, 4).reshape(B, nc, G, CMP_BLOCK * dk)
    return jax.nn.gelu(blk @ w1) @ w2


def nsa_mixer(q, k_cmp, v_cmp, k_sel, v_sel, k_win, v_win, gates, rel_bias,
              q_norm, k_norm, cmp_pos, cmp_w1, cmp_w2):
    B, S = q.shape[0], q.shape[1]
    G, R, dk = NSA_KV_GROUPS, NSA_HEADS // NSA_KV_GROUPS, NSA_HEAD_DIM
    QB, L = NSA_Q_BLOCK, WINDOW + NSA_Q_BLOCK
    scale = dk ** -0.5
    qn = rms_norm(q, q_norm).reshape(B, S, G, R, dk)
    kc = rms_norm(compress(k_cmp, cmp_pos[0], cmp_w1[0], cmp_w2[0]), k_norm[0])
    vc = compress(v_cmp, cmp_pos[1], cmp_w1[1], cmp_w2[1])
    nc = kc.shape[1]
    ns = S // SEL_BLOCK
    n_top = min(SEL_TOP_N, ns)

    def to_blocks(a):
        return a.reshape(B, ns, SEL_BLOCK, G, dk).transpose(0, 3, 1, 2, 4).reshape(B, G, ns, SEL_BLOCK * dk)

    ks_blk = to_blocks(rms_norm(k_sel, k_norm[1]))
    vs_blk = to_blocks(v_sel)
    pad = ((0, 0), (WINDOW, 0), (0, 0), (0, 0))
    kw_pad = jnp.pad(rms_norm(k_win, k_norm[2]), pad)
    vw_pad = jnp.pad(v_win, pad)
    gate_all = gates.reshape(B, S, G, R, 3)

    cmp_start = jnp.arange(nc) * CMP_STRIDE
    cmp_end = cmp_start + CMP_BLOCK - 1
    sel_start = jnp.arange(ns) * SEL_BLOCK
    overlap = ((cmp_start[:, None] < sel_start[None, :] + SEL_BLOCK)
               & (cmp_end[:, None] >= sel_start[None, :])).astype(jnp.float32)
    bias_gr = rel_bias.reshape(REL_BUCKETS, G, R)
    g_idx = jnp.arange(G)[None, :, None, None, None]
    blk_ids = jnp.arange(ns)
    offs_sel = jnp.arange(SEL_BLOCK)
    flat = n_top * SEL_BLOCK

    def one_block(i):
        q0 = i * QB
        t = q0 + jnp.arange(QB)
        qb = lax.dynamic_slice_in_dim(qn, q0, QB, axis=1)
        d_c = t[:, None] - cmp_end[None, :]
        s_c = jnp.einsum('bqgrd,bcgd->bgrqc', qb, kc).astype(jnp.float32) * scale
        s_c = s_c + bias_gr[t5_bucket(d_c)].transpose(2, 3, 0, 1).astype(jnp.float32)
        p_c = masked_softmax(s_c, d_c >= 0)
        o_c = jnp.einsum('bgrqc,bcgd->bqgrd', p_c.astype(vc.dtype), vc)
        imp = jnp.einsum('bgrqc,cj->bgqj', p_c, overlap)
        forced = (blk_ids[None, :] == (t // SEL_BLOCK)[:, None]) | (blk_ids[None, :] == 0)
        imp = jnp.where(forced, FORCED_SCORE, imp)
        imp = jnp.where(sel_start[None, :] <= t[:, None], imp, -1.0)
        _, sel = lax.top_k(imp, n_top)
        sel_flat = sel.reshape(B, G, QB * n_top, 1)
        k_g = jnp.take_along_axis(ks_blk, sel_flat, axis=2).reshape(B, G, QB, n_top, SEL_BLOCK, dk)
        v_g = jnp.take_along_axis(vs_blk, sel_flat, axis=2).reshape(B, G, QB, flat, dk)
        s_pos = sel[..., None] * SEL_BLOCK + offs_sel
        d_s = t[None, None, :, None, None] - s_pos
        s_s = jnp.einsum('bqgrd,bgqnkd->bgrqnk', qb, k_g).astype(jnp.float32) * scale
        s_s = s_s + jnp.moveaxis(bias_gr[t5_bucket(d_s), g_idx], -1, 2).astype(jnp.float32)
        p_s = masked_softmax(s_s.reshape(B, G, R, QB, flat), (d_s >= 0).reshape(B, G, 1, QB, flat))
        o_s = jnp.einsum('bgrqk,bgqkd->bqgrd', p_s.astype(v_g.dtype), v_g)
        k_w = lax.dynamic_slice_in_dim(kw_pad, q0, L, axis=1)
        v_w = lax.dynamic_slice_in_dim(vw_pad, q0, L, axis=1)
        w_pos = q0 - WINDOW + jnp.arange(L)
        d_w = t[:, None] - w_pos[None, :]
        s_w = jnp.einsum('bqgrd,bkgd->bgrqk', qb, k_w).astype(jnp.float32) * scale
        s_w = s_w + bias_gr[t5_bucket(d_w)].transpose(2, 3, 0, 1).astype(jnp.float32)
        p_w = masked_softmax(s_w, (d_w >= 0) & (d_w < WINDOW) & (w_pos[None, :] >= 0))
        o_w = jnp.einsum('bgrqk,bkgd->bqgrd', p_w.astype(v_w.dtype), v_w)
        g = jax.nn.sigmoid(lax.dynamic_slice_in_dim(gate_all, q0, QB, axis=1).astype(jnp.float32))
        o = g[..., 0:1] * o_c + g[..., 1:2] * o_s + g[..., 2:3] * o_w
        return o.astype(q.dtype)

    out = lax.map(one_block, jnp.arange(S // QB))
    return out.transpose(1, 0, 2, 3, 4, 5).reshape(B, S, NSA_HEADS * dk)


def causal_block_attention(q, k, v):
    B, S, H, dh = q.shape
    scale = dh ** -0.5
    key_pos = jnp.arange(S)

    def one_block(i):
        q0 = i * ATTN_Q_BLOCK
        qb = lax.dynamic_slice_in_dim(q, q0, ATTN_Q_BLOCK, axis=1)
        s = jnp.einsum('bqhd,bkhd->bhqk', qb, k).astype(jnp.float32) * scale
        mask = (q0 + jnp.arange(ATTN_Q_BLOCK))[:, None] >= key_pos[None, :]
        p = masked_softmax(s, mask)
        return jnp.einsum('bhqk,bkhd->bqhd', p.astype(v.dtype), v)

    out = lax.map(one_block, jnp.arange(S // ATTN_Q_BLOCK))
    return out.transpose(1, 0, 2, 3, 4).reshape(B, S, H, v.shape[-1])


def mla_mixer(c_q, c_kv, k_rope, cq_norm, ckv_norm, w_uq, w_ukv, q_norm, k_norm):
    B, S = c_q.shape[0], c_q.shape[1]
    H, dn, dr, dv = MLA_HEADS, MLA_NOPE_DIM, MLA_ROPE_DIM, MLA_V_DIM
    pos = jnp.arange(S)
    q = (rms_norm(c_q, cq_norm) @ w_uq).reshape(B, S, H, dn + dr)
    kv = (rms_norm(c_kv, ckv_norm) @ w_ukv).reshape(B, S, H, dn + dv)
    k = jnp.concatenate([kv[..., :dn], jnp.broadcast_to(k_rope[:, :, None, :], (B, S, H, dr))], axis=-1)
    v = kv[..., dn:]
    q = rms_norm(q, q_norm)
    k = rms_norm(k, k_norm)
    q = jnp.concatenate([q[..., :dn], rope(q[..., dn:], pos)], axis=-1)
    k = jnp.concatenate([k[..., :dn], rope(k[..., dn:], pos)], axis=-1)
    return causal_block_attention(q, k, v).reshape(B, S, H * dv)


def short_conv_mixer(u, b_gate, c_gate, conv_w):
    y = lax.conv_general_dilated(c_gate * u, conv_w[:, None, :], window_strides=(1,),
                                 padding=[(CONV_WIDTH - 1, 0)],
                                 dimension_numbers=('NWC', 'WIO', 'NWC'),
                                 feature_group_count=u.shape[-1])
    return b_gate * y


def swiglu(x, w_gate, w_up, w_down):
    return (jax.nn.silu(x @ w_gate) * (x @ w_up)) @ w_down


def moe_swiglu(x, w_router, b_router, w_gate, w_up, w_down):
    B, S, D = x.shape
    T = B * S
    RB = MOE_ROW_BLOCK
    xt = x.reshape(T, D)
    logits = (xt @ w_router).astype(jnp.float32) + b_router.astype(jnp.float32)
    top_logit, top_e = lax.top_k(logits, TOP_K)
    top_w = jax.nn.softmax(top_logit, axis=-1)
    P = T * TOP_K
    e_flat = top_e.reshape(P)
    tok_flat = jnp.arange(P) // TOP_K
    w_flat = top_w.reshape(P)
    order = jnp.argsort(e_flat)
    e_s, tok_s, w_s = e_flat[order], tok_flat[order], w_flat[order]
    counts = jnp.bincount(e_flat, length=N_EXPERTS)
    padded = (counts + RB - 1) // RB * RB
    start = jnp.cumsum(counts) - counts
    pad_end = jnp.cumsum(padded)
    pad_start = pad_end - padded
    dest = pad_start[e_s] + jnp.arange(P) - start[e_s]
    n_blk = (P + RB - 1) // RB + N_EXPERTS
    n_rows = n_blk * RB
    tok_buf = jnp.zeros((n_rows,), jnp.int32).at[dest].set(tok_s)
    w_buf = jnp.zeros((n_rows,), jnp.float32).at[dest].set(w_s)
    blk_expert = jnp.minimum(jnp.sum(pad_end[None, :] <= (jnp.arange(n_blk) * RB)[:, None], axis=1),
                             N_EXPERTS - 1)

    def one_block(args):
        e, toks, w = args
        xb = xt[toks]
        h = jax.nn.silu(xb @ w_gate[e]) * (xb @ w_up[e])
        return (h @ w_down[e]) * w[:, None].astype(h.dtype)

    out = lax.map(one_block, (blk_expert, tok_buf.reshape(n_blk, RB), w_buf.reshape(n_blk, RB)))
    y = jnp.zeros((T, D), x.dtype).at[tok_buf].add(out.reshape(n_rows, D).astype(x.dtype))
    return y.reshape(B, S, D)


def setup_inputs(seed: int = 0) -> dict:
    key = jax.random.key(seed)
    keys = iter(jax.random.split(key, 32))
    ne, no = (DEPTH + 1) // 2, DEPTH // 2

    def dense(shape, fan_in):
        return jax.random.normal(next(keys), shape, jnp.float32) * (fan_in ** -0.5)

    def gain(shape):
        return 1.0 + 0.02 * jax.random.normal(next(keys), shape, jnp.float32)

    def small(shape, scale):
        return scale * jax.random.normal(next(keys), shape, jnp.float32)

    cmp_in = CMP_BLOCK * NSA_HEAD_DIM
    mla_qk = MLA_NOPE_DIM + MLA_ROPE_DIM
    return {
        'x': jax.random.normal(next(keys), (BATCH, SEQ, D_MODEL), jnp.float32),
        'rel_bias': small((REL_BUCKETS, NSA_HEADS), 0.1),
        'ev_mix_norm': gain((ne, D_MODEL)),
        'ev_w_in': dense((ne, D_MODEL, EVEN_IN_W), D_MODEL),
        'nsa_q_norm': gain((ne, NSA_HEAD_DIM)),
        'nsa_k_norm': gain((ne, 3, NSA_HEAD_DIM)),
        'nsa_cmp_pos': small((ne, 2, CMP_BLOCK, NSA_HEAD_DIM), 0.02),
        'nsa_cmp_w1': dense((ne, 2, cmp_in, CMP_HIDDEN), cmp_in),
        'nsa_cmp_w2': dense((ne, 2, CMP_HIDDEN, NSA_HEAD_DIM), CMP_HIDDEN),
        'mla_cq_norm': gain((ne, MLA_Q_RANK)),
        'mla_ckv_norm': gain((ne, MLA_KV_RANK)),
        'mla_w_uq': dense((ne, MLA_Q_RANK, MLA_HEADS * mla_qk), MLA_Q_RANK),
        'mla_w_ukv': dense((ne, MLA_KV_RANK, MLA_HEADS * (MLA_NOPE_DIM + MLA_V_DIM)), MLA_KV_RANK),
        'mla_q_norm': gain((ne, mla_qk)),
        'mla_k_norm': gain((ne, mla_qk)),
        'ev_w_out': dense((ne, MIX_OUT_W, D_MODEL), MIX_OUT_W),
        'ev_ffn_norm': gain((ne, D_MODEL)),
        'ffn_w_gate': dense((ne, D_MODEL, D_FF), D_MODEL),
        'ffn_w_up': dense((ne, D_MODEL, D_FF), D_MODEL),
        'ffn_w_down': dense((ne, D_FF, D_MODEL), D_FF),
        'od_mix_norm': gain((no, D_MODEL)),
        'od_w_in': dense((no, D_MODEL, 3 * D_MODEL), D_MODEL),
        'conv_w': dense((no, CONV_WIDTH, D_MODEL), CONV_WIDTH),
        'od_w_out': dense((no, D_MODEL, D_MODEL), D_MODEL),
        'od_ffn_norm': gain((no, D_MODEL)),
        'moe_w_router': dense((no, D_MODEL, N_EXPERTS), D_MODEL),
        'moe_b_router': small((no, N_EXPERTS), 0.01),
        'moe_w_gate': dense((no, N_EXPERTS, D_MODEL, D_FF_EXPERT), D_MODEL),
        'moe_w_up': dense((no, N_EXPERTS, D_MODEL, D_FF_EXPERT), D_MODEL),
        'moe_w_down': dense((no, N_EXPERTS, D_FF_EXPERT, D_MODEL), D_FF_EXPERT),
    }


def reference(x, rel_bias, ev_mix_norm, ev_w_in, nsa_q_norm, nsa_k_norm, nsa_cmp_pos, nsa_cmp_w1,
              nsa_cmp_w2, mla_cq_norm, mla_ckv_norm, mla_w_uq, mla_w_ukv, mla_q_norm, mla_k_norm,
              ev_w_out, ev_ffn_norm, ffn_w_gate, ffn_w_up, ffn_w_down, od_mix_norm, od_w_in, conv_w,
              od_w_out, od_ffn_norm, moe_w_router, moe_b_router, moe_w_gate, moe_w_up, moe_w_down):
    B, S, _ = x.shape
    split_at = np.cumsum(EVEN_IN_SIZES)[:-1].tolist()
    kv_shape = (B, S, NSA_KV_GROUPS, NSA_HEAD_DIM)
    for layer in range(DEPTH):
        i = layer // 2
        if layer % 2 == 0:
            h = rms_norm(x, ev_mix_norm[i]) @ ev_w_in[i]
            (q, k_c, v_c, k_s, v_s, k_w, v_w, gates, c_q, c_kv, k_rope) = jnp.split(h, split_at, axis=-1)
            o_nsa = nsa_mixer(q.reshape(B, S, NSA_HEADS, NSA_HEAD_DIM),
                              k_c.reshape(kv_shape), v_c.reshape(kv_shape),
                              k_s.reshape(kv_shape), v_s.reshape(kv_shape),
                              k_w.reshape(kv_shape), v_w.reshape(kv_shape),
                              gates, rel_bias, nsa_q_norm[i], nsa_k_norm[i],
                              nsa_cmp_pos[i], nsa_cmp_w1[i], nsa_cmp_w2[i])
            o_mla = mla_mixer(c_q, c_kv, k_rope, mla_cq_norm[i], mla_ckv_norm[i],
                              mla_w_uq[i], mla_w_ukv[i], mla_q_norm[i], mla_k_norm[i])
            x = x + jnp.concatenate([o_nsa, o_mla], axis=-1) @ ev_w_out[i]
            x = x + swiglu(rms_norm(x, ev_ffn_norm[i]), ffn_w_gate[i], ffn_w_up[i], ffn_w_down[i])
        else:
            b_g, c_g, u = jnp.split(rms_norm(x, od_mix_norm[i]) @ od_w_in[i], 3, axis=-1)
            x = x + short_conv_mixer(u, b_g, c_g, conv_w[i]) @ od_w_out[i]
            x = x + moe_swiglu(rms_norm(x, od_ffn_norm[i]), moe_w_router[i], moe_b_router[i],
                               moe_w_gate[i], moe_w_up[i], moe_w_down[i])
    return x
```

```python
import math
from contextlib import ExitStack, contextmanager
import numpy as np
import concourse.bass as bass
import concourse.mybir as mybir
from concourse.bass_utils import run_bass_kernel_spmd

F32, BF16 = mybir.dt.float32, mybir.dt.bfloat16
AF = mybir.ActivationFunctionType
ALU = mybir.AluOpType
AX = mybir.AxisListType

S = 4096
D = 1024
NT = S // 128
EPS = 1e-6
EVEN_W = 1720
D_FF = 2816
NE = 8
DFE = 1408


class Buf:
    def __init__(self, name, h, multi=False):
        self.name = name
        self.h = h
        self.w = None
        self.wm = {} if multi else None
        self.r = {}
        self.lane = None

    def __getitem__(self, key):
        return self.h[key]


class KB:
    def __init__(self, nc, es, n_lanes=72):
        self.nc = nc
        self.eng = {'pe': nc.tensor, 'act': nc.scalar, 'dve': nc.vector, 'pool': nc.gpsimd, 'sp': nc.sync}
        self.sems = {}
        self.cnt = {}
        for e in ('pe', 'act', 'dve', 'pool'):
            self.sems[e] = es.enter_context(nc.semaphore('sem_' + e))
            self.cnt[e] = 0
        self.free_lanes = []
        self.free_sw_lanes = []
        for i in range(n_lanes):
            key = 'lane%d' % i
            self.sems[key] = es.enter_context(nc.semaphore(key))
            self.cnt[key] = 0
            (self.free_sw_lanes if i < 16 else self.free_lanes).append(key)
        self.seen = {e: {} for e in self.eng}
        self.phase_bufs = []
        self.uid = 0

    def _need(self, e, stamp):
        if stamp is None:
            return
        sk, v, _ = stamp
        if self.seen[e].get(sk, 0) >= v:
            return
        self.eng[e].wait_ge(self.sems[sk], v)
        self.seen[e][sk] = v

    def _deps(self, e, reads, writes, is_dma=False):
        for b in reads:
            self._need(e, b.w)
            if b.wm:
                for st in b.wm.values():
                    self._need(e, st)
        for b in writes:
            if b.w is not None and (is_dma or b.w[2] != e or e != 'pe'):
                self._need(e, b.w)
            for st in b.r.values():
                if is_dma or st[2] != e or e != 'pe':
                    self._need(e, st)

    def op(self, e, reads, writes, fn):
        self._deps(e, reads, writes)
        ins = fn(self.eng[e])
        self.cnt[e] += 1
        ins.then_inc(self.sems[e], 1)
        st = (e, self.cnt[e], e)
        for b in reads:
            b.r[e] = st
        for b in writes:
            b.w = st
            b.r = {}
        return ins

    def dma(self, q, out, in_, reads, writes, owner, **kw):
        self._deps(q, reads, writes, is_dma=True)
        sw = q == 'pool'
        attr = 'lane_sw' if sw else 'lane'
        if getattr(owner, attr, None) is None:
            lane = (self.free_sw_lanes if sw else self.free_lanes).pop()
            setattr(owner, attr, lane)
            (self.phase_sw_lanes if sw else self.phase_lanes).append(lane)
        lk = getattr(owner, attr)
        self.eng[q].dma_start(out=out, in_=in_, **kw).then_inc(self.sems[lk], 16)
        self.cnt[lk] += 16
        st = (lk, self.cnt[lk], None)
        for b in reads:
            b.r[lk] = st
        for b in writes:
            if b.wm is not None:
                b.wm[lk] = st
            else:
                b.w = st
                b.r = {}

    def barrier(self):
        keys = [k for k in self.sems if self.cnt[k] > 0]
        for e in self.eng:
            for k in keys:
                if k == e:
                    continue
                self._need(e, (k, self.cnt[k], None))

    @contextmanager
    def phase(self):
        self.phase_lanes = []
        self.phase_sw_lanes = []
        with ExitStack() as es:
            ph = Phase(self, es)
            yield ph
            self.barrier()
        self.free_lanes.extend(self.phase_lanes)
        self.free_sw_lanes.extend(self.phase_sw_lanes)
        self.phase_lanes = []
        self.phase_sw_lanes = []


class Phase:
    def __init__(self, kb, es):
        self.kb = kb
        self.es = es

    def sb(self, name, shape, dt):
        self.kb.uid += 1
        h = self.es.enter_context(self.kb.nc.sbuf_tensor('%s_%d' % (name, self.kb.uid), shape, dt))
        return Buf(name, h)

    def ps(self, name, shape, dt):
        self.kb.uid += 1
        h = self.es.enter_context(self.kb.nc.psum_tensor('%s_%d' % (name, self.kb.uid), shape, dt))
        return Buf(name, h)


def bcast_mid(ap2d_rows, n):
    return ap2d_rows.unsqueeze(2).broadcast_to([ap2d_rows.shape[0], ap2d_rows.shape[1], n])


class Ctx:
    pass


def load_w(kb, ph, dst, src, K, N, stage=None, rowscale=None, **kw):
    assert rowscale is None
    for kc in range(K // 128):
        kb.dma('pool', dst[:, kc, :], src[kc * 128:(kc + 1) * 128, :], [], [dst], dst)


def rstd_from_ssq(kb, ssq, rstd, n, width):
    kb.op('act', [ssq], [rstd], lambda en: en.activation(out=rstd[:, 0:width], in_=ssq[:, 0:width], func=AF.Sqrt,
                                                         bias=EPS, scale=1.0 / n))
    kb.op('dve', [rstd], [rstd], lambda en: en.vector.reciprocal(out=rstd[:, 0:width], in_=rstd[:, 0:width])
          if False else en.reciprocal(out=rstd[:, 0:width], in_=rstd[:, 0:width]))


def ring(ph, name, shape, dt, n=2):
    return [ph.sb('%s%d' % (name, i), shape, dt) for i in range(n)]


def bc_row(t, n):
    return bass.AP(t, 0, [[0, 128], [1, n]])


def phase_e1(kb, d, nt=NT, lim=99):
    with kb.phase() as ph:
        identf = ph.sb('identf', [128, 128], F32)
        ident = ph.sb('ident', [128, 128], BF16)
        kb.dma('sp', identf[:], d['c_ident'][:, :], [], [identf], identf)
        kb.op('dve', [identf], [ident], lambda en: en.tensor_copy(out=ident[:], in_=identf[:]))
        Win = ph.sb('Win', [128, 8, EVEN_W], BF16)
        Wuq = ph.sb('Wuq', [128, 2, 768], BF16)
        Wukv = ph.sb('Wukv', [128, 1, 1024], BF16)
        qcol = ph.sb('qcol', [64, 1], F32)
        kcol = ph.sb('kcol', [64, 3], F32)
        mqg = ph.sb('mqg', [128, 768], F32)
        mkg = ph.sb('mkg', [128, 96], F32)
        for t, src in ((qcol, d['nsa_q_col']), (kcol, d['nsa_k_col'])):
            kb.dma('sp', t[:], src[:, :], [], [t], t)
        grow = ph.sb('grow', [128, D], F32)
        crow = ph.sb('crow', [128, 384], F32)
        kb.dma('sp', grow[:], bc_row(d['ev_mix_row'], D), [], [grow], grow)
        kb.dma('sp', crow[:], bc_row(d['cqkv_row'], 384), [], [crow], crow)
        kb.dma('sp', mqg[:], bc_row(d['mla_q_row'], 768), [], [mqg], mqg)
        kb.dma('sp', mkg[:], bc_row(d['mla_k_row'], 96), [], [mkg], mkg)
        kb.op('dve', [qcol], [qcol], lambda en: en.tensor_scalar(out=qcol[:], in0=qcol[:], scalar1=64 ** -0.5,
                                                                  scalar2=None, op0=ALU.mult))
        load_w(kb, ph, Win, d['ev_w_in'], 1024, EVEN_W)
        load_w(kb, ph, Wuq, d['mla_w_uq'], 256, 768)
        load_w(kb, ph, Wukv, d['mla_w_ukv'], 128, 1024)

        T0 = ph.ps('T0', [128, 8, 128], BF16)
        T1 = ph.ps('T1', [128, 8, 128], BF16)
        M = [ph.ps('M%d' % i, [128, 512], F32) for i in range(6)]
        xr = ring(ph, 'xr', [128, D], F32)
        junk = ph.sb('junk', [128, D], BF16)
        ssq = ring(ph, 'ssq', [128, 16], F32)
        rstd = ring(ph, 'rstd', [128, 16], F32)
        xs = ring(ph, 'xs', [128, D], BF16)
        xnT = ring(ph, 'xnT', [128, 8, 128], BF16)
        sq = ring(ph, 'sq', [128, 768], F32)
        qn = ring(ph, 'qn', [128, 8, 64], BF16)
        kn = ring(ph, 'kn', [128, 4, 64], BF16)
        cb = ring(ph, 'cb', [128, 256], BF16)
        vb = ring(ph, 'vb', [128, 4, 64], BF16)
        gt = ring(ph, 'gt', [128, 24], F32)
        cqn = ring(ph, 'cqn', [128, 384], BF16)
        krp = ring(ph, 'krp', [128, 32], F32)
        cT = ring(ph, 'cT', [128, 3, 128], BF16)
        qf = ring(ph, 'qf', [128, 8, 96], F32)
        rt = ring(ph, 'rt', [128, 4, 8, 16], F32)
        qb = ring(ph, 'qb', [128, 8, 96], BF16)
        kbf = ring(ph, 'kbf', [128, 8, 96], BF16)
        kt = ring(ph, 'kt', [128, 8, 64], F32)
        kr2 = ring(ph, 'kr2', [128, 2, 32], F32)
        vb2 = ring(ph, 'vb2', [128, 8, 64], BF16)
        cs = ring(ph, 'cs', [128, 32], F32)
        QT = ring(ph, 'QT', [64, 8, 512], BF16)
        KST = ring(ph, 'KST', [64, 4, 512], BF16)
        CT = ring(ph, 'CT', [128, 2, 512], BF16)
        MQT = ring(ph, 'MQT', [96, 8, 512], BF16)
        MKT = ring(ph, 'MKT', [96, 8, 512], BF16)
        chunks = ((0, 512), (512, 1024), (1024, 1304), (1304, 1720))

        for tt in range(nt):
            st, j = divmod(tt, 4)
            r = tt % 2
            s2 = st % 2
            cols = slice(j * 128, (j + 1) * 128)
            rows = slice(tt * 128, (tt + 1) * 128)
            x_, ssq_, rstd_, xs_, xnT_, sq_ = xr[r], ssq[r], rstd[r], xs[r], xnT[r], sq[r]
            kb.dma('sp', x_[:], d['x'][rows, :], [], [x_], x_)
            kb.dma('act', cs[r][:], d['c_cossin'][rows, :], [], [cs[r]], cs[r])
            if lim < 1:
                continue
            kb.op('pool', [], [ssq_], lambda en: en.memset(ssq_[:], 0.0))
            kb.op('act', [x_], [junk, ssq_], lambda en: en.activation(out=junk[:], in_=x_[:], func=AF.Square,
                                                                      accum_out=ssq_[:, 0:1]))
            kb.op('act', [ssq_], [rstd_], lambda en: en.activation(out=rstd_[:, 0:1], in_=ssq_[:, 0:1], func=AF.Sqrt,
                                                                   bias=EPS, scale=1.0 / D))
            kb.op('dve', [rstd_], [rstd_], lambda en: en.reciprocal(out=rstd_[:, 0:1], in_=rstd_[:, 0:1]))
            kb.op('dve', [x_, rstd_, grow], [xs_], lambda en: en.scalar_tensor_tensor(out=xs_[:], in0=x_[:], scalar=rstd_[:, 0:1], in1=grow[:],
                                                                                     op0=ALU.mult, op1=ALU.mult))
            if lim < 2:
                continue
            for k in range(8):
                kb.op('pe', [xs_, ident], [T0], lambda en, k=k: en.transpose(out=T0[:, k, :], in_=xs_[:, k * 128:(k + 1) * 128],
                                                                             identity=ident[:]))
            kb.op('act', [T0], [xnT_], lambda en: en.copy(out=xnT_[:], in_=T0[:]))
            if lim < 3:
                continue
            for ci, (c0, c1) in enumerate(chunks):
                for k in range(8):
                    kb.op('pe', [xnT_, Win], [M[ci]], lambda en, k=k, ci=ci, c0=c0, c1=c1: en.matmul(
                        M[ci][:, 0:c1 - c0], lhsT=xnT_[:, k, :], rhs=Win[:, k, c0:c1], start=(k == 0), stop=(k == 7)))
            hA, hB, hC, hD = M[0], M[1], M[2], M[3]
            if lim < 4:
                continue
            kb.op('act', [hA], [sq_], lambda en: en.activation(out=sq_[:, 0:512], in_=hA[:, 0:512], func=AF.Square))
            kb.op('dve', [sq_], [ssq_], lambda en: en.tensor_reduce(out=ssq_[:, 1:9], in_=sq_[:, 0:512].rearrange(
                'p (h d) -> p h d', h=8), axis=AX.X, op=ALU.add))
            kb.op('act', [ssq_], [rstd_], lambda en: en.activation(out=rstd_[:, 1:9], in_=ssq_[:, 1:9], func=AF.Sqrt,
                                                                   bias=EPS, scale=1.0 / 64))
            kb.op('dve', [rstd_], [rstd_], lambda en: en.reciprocal(out=rstd_[:, 1:9], in_=rstd_[:, 1:9]))
            qn_ = qn[r]
            kb.op('dve', [hA, rstd_], [qn_], lambda en: en.tensor_tensor(
                out=qn_[:], in0=hA[:, 0:512].rearrange('p (h d) -> p h d', h=8), in1=bcast_mid(rstd_[:, 1:9], 64), op=ALU.mult))
            for h in range(8):
                kb.op('pe', [qn_, ident], [T1], lambda en, h=h: en.transpose(out=T1[0:64, h, :], in_=qn_[:, h, :], identity=ident[:]))
            QT_ = QT[s2]
            kb.op('act', [T1, qcol], [QT_], lambda en: en.activation(out=QT_[:, :, cols], in_=T1[0:64, :, :], func=AF.Copy,
                                                                     scale=qcol[:, 0:1]))
            if lim < 5:
                continue
            kb.op('act', [hB], [sq_], lambda en: en.activation(out=sq_[:, 0:128], in_=hB[:, 256:384], func=AF.Square))
            kb.op('act', [hC], [sq_], lambda en: en.activation(out=sq_[:, 128:256], in_=hC[:, 0:128], func=AF.Square))
            kb.op('dve', [sq_], [ssq_], lambda en: en.tensor_reduce(out=ssq_[:, 9:13], in_=sq_[:, 0:256].rearrange(
                'p (h d) -> p h d', h=4), axis=AX.X, op=ALU.add))
            kb.op('act', [ssq_], [rstd_], lambda en: en.activation(out=rstd_[:, 9:13], in_=ssq_[:, 9:13], func=AF.Sqrt,
                                                                   bias=EPS, scale=1.0 / 64))
            kb.op('dve', [rstd_], [rstd_], lambda en: en.reciprocal(out=rstd_[:, 9:13], in_=rstd_[:, 9:13]))
            kn_ = kn[r]
            kb.op('dve', [hB, rstd_], [kn_], lambda en: en.tensor_tensor(
                out=kn_[:, 0:2, :], in0=hB[:, 256:384].rearrange('p (h d) -> p h d', h=2), in1=bcast_mid(rstd_[:, 9:11], 64), op=ALU.mult))
            kb.op('dve', [hC, rstd_], [kn_], lambda en: en.tensor_tensor(
                out=kn_[:, 2:4, :], in0=hC[:, 0:128].rearrange('p (h d) -> p h d', h=2), in1=bcast_mid(rstd_[:, 11:13], 64), op=ALU.mult))
            cb_ = cb[r]
            kb.op('dve', [hB], [cb_], lambda en: en.tensor_copy(out=cb_[:], in_=hB[:, 0:256]))
            vb_ = vb[r]
            kb.op('act', [hB], [vb_], lambda en: en.copy(out=vb_[:, 0:2, :], in_=hB[:, 384:512].rearrange('p (h d) -> p h d', h=2)))
            kb.op('act', [hC], [vb_], lambda en: en.copy(out=vb_[:, 2:4, :], in_=hC[:, 128:256].rearrange('p (h d) -> p h d', h=2)))
            kb.dma('sp', d['nsa_v'][rows, :, :], vb_[:], [vb_], [d['nsa_v_b']], vb_)
            gt_ = gt[r]
            kb.op('act', [hC], [gt_], lambda en: en.activation(out=gt_[:], in_=hC[:, 256:280], func=AF.Sigmoid))
            kb.dma('sp', d['gates'][rows, :], gt_[:], [gt_], [d['gates_b']], gt_)
            for i in range(4):
                kb.op('pe', [kn_, ident], [T0], lambda en, i=i: en.transpose(out=T0[0:64, i, :], in_=kn_[:, i, :], identity=ident[:]))
            for i in range(2):
                kb.op('pe', [cb_, ident], [T0], lambda en, i=i: en.transpose(out=T0[:, 4 + i, :], in_=cb_[:, i * 128:(i + 1) * 128],
                                                                             identity=ident[:]))
            KST_ = KST[s2]
            kb.op('act', [T0, kcol], [KST_], lambda en: en.activation(out=KST_[:, 0:2, cols], in_=T0[0:64, 0:2, :], func=AF.Copy,
                                                                      scale=kcol[:, 1:2]))
            kb.op('act', [T0, kcol], [KST_], lambda en: en.activation(out=KST_[:, 2:4, cols], in_=T0[0:64, 2:4, :], func=AF.Copy,
                                                                      scale=kcol[:, 2:3]))
            CT_ = CT[s2]
            kb.op('act', [T0], [CT_], lambda en: en.copy(out=CT_[:, :, cols], in_=T0[:, 4:6, :]))
            if lim < 6:
                continue
            kb.op('pool', [], [ssq_], lambda en: en.memset(ssq_[:, 13:16], 0.0))
            kb.op('act', [hD], [sq_, ssq_], lambda en: en.activation(out=sq_[:, 0:256], in_=hD[:, 0:256], func=AF.Square,
                                                                     accum_out=ssq_[:, 13:14]))
            kb.op('act', [hD], [sq_, ssq_], lambda en: en.activation(out=sq_[:, 256:384], in_=hD[:, 256:384], func=AF.Square,
                                                                     accum_out=ssq_[:, 14:15]))
            kb.op('act', [hD], [sq_, ssq_], lambda en: en.activation(out=sq_[:, 384:416], in_=hD[:, 384:416], func=AF.Square,
                                                                     accum_out=ssq_[:, 15:16]))
            kb.op('act', [ssq_], [rstd_], lambda en: en.activation(out=rstd_[:, 13:14], in_=ssq_[:, 13:14], func=AF.Sqrt,
                                                                   bias=EPS, scale=1.0 / 256))
            kb.op('act', [ssq_], [rstd_], lambda en: en.activation(out=rstd_[:, 14:15], in_=ssq_[:, 14:15], func=AF.Sqrt,
                                                                   bias=EPS, scale=1.0 / 128))
            kb.op('dve', [rstd_], [rstd_], lambda en: en.reciprocal(out=rstd_[:, 13:15], in_=rstd_[:, 13:15]))
            cqn_ = cqn[r]
            kb.op('dve', [hD, rstd_, crow], [cqn_], lambda en: en.scalar_tensor_tensor(out=cqn_[:, 0:256], in0=hD[:, 0:256], scalar=rstd_[:, 13:14],
                                                                                      in1=crow[:, 0:256], op0=ALU.mult, op1=ALU.mult))
            kb.op('dve', [hD, rstd_, crow], [cqn_], lambda en: en.scalar_tensor_tensor(out=cqn_[:, 256:384], in0=hD[:, 256:384], scalar=rstd_[:, 14:15],
                                                                                      in1=crow[:, 256:384], op0=ALU.mult, op1=ALU.mult))
            krp_ = krp[r]
            kb.op('dve', [hD, mkg], [krp_], lambda en: en.tensor_tensor(out=krp_[:], in0=hD[:, 384:416], in1=mkg[:, 64:96], op=ALU.mult))
            for i in range(3):
                kb.op('pe', [cqn_, ident], [T1], lambda en, i=i: en.transpose(out=T1[:, i, :], in_=cqn_[:, i * 128:(i + 1) * 128],
                                                                              identity=ident[:]))
            cT_ = cT[r]
            kb.op('act', [T1], [cT_], lambda en: en.copy(out=cT_[:], in_=T1[:, 0:3, :]))
            for c in range(2):
                for k in range(2):
                    kb.op('pe', [cT_, Wuq], [M[4 + c]], lambda en, c=c, k=k: en.matmul(
                        M[4 + c][:, 0:384], lhsT=cT_[:, k, :], rhs=Wuq[:, k, c * 384:(c + 1) * 384], start=(k == 0), stop=(k == 1)))
            for c in range(2):
                kb.op('act', [M[4 + c]], [sq_], lambda en, c=c: en.activation(out=sq_[:, c * 384:(c + 1) * 384], in_=M[4 + c][:, 0:384],
                                                                              func=AF.Square))
            kb.op('dve', [sq_], [ssq_], lambda en: en.tensor_reduce(out=ssq_[:, 1:9], in_=sq_[:, 0:768].rearrange(
                'p (h d) -> p h d', h=8), axis=AX.X, op=ALU.add))
            kb.op('act', [ssq_], [rstd_], lambda en: en.activation(out=rstd_[:, 1:9], in_=ssq_[:, 1:9], func=AF.Sqrt,
                                                                   bias=EPS, scale=1.0 / 96))
            kb.op('dve', [rstd_], [rstd_], lambda en: en.reciprocal(out=rstd_[:, 1:9], in_=rstd_[:, 1:9]))
            qf_ = qf[r]
            for c in range(2):
                kb.op('dve', [M[4 + c], rstd_], [qf_], lambda en, c=c: en.tensor_tensor(
                    out=qf_[:, c * 4:(c + 1) * 4, :], in0=M[4 + c][:, 0:384].rearrange('p (h d) -> p h d', h=4),
                    in1=bcast_mid(rstd_[:, 1 + c * 4:5 + c * 4], 96), op=ALU.mult))
            kb.op('pool', [qf_, mqg], [qf_], lambda en: en.tensor_tensor(out=qf_[:], in0=qf_[:], in1=mqg[:].rearrange(
                'p (h d) -> p h d', h=8), op=ALU.mult))
            cs_ = cs[r]
            rt_ = rt[r]
            qb_ = qb[r]

            def rope_bc(a, nh):
                return a.unsqueeze(1).broadcast_to([128, nh, 16])
            cosb, sinb = rope_bc(cs_[:, 0:16], 8), rope_bc(cs_[:, 16:32], 8)
            x1, x2 = qf_[:, :, 64:80], qf_[:, :, 80:96]
            kb.op('pool', [qf_, cs_], [rt_], lambda en: en.tensor_tensor(out=rt_[:, 0], in0=x1, in1=cosb, op=ALU.mult))
            kb.op('pool', [qf_, cs_], [rt_], lambda en: en.tensor_tensor(out=rt_[:, 1], in0=x2, in1=sinb, op=ALU.mult))
            kb.op('dve', [qf_, cs_], [rt_], lambda en: en.tensor_tensor(out=rt_[:, 2], in0=x1, in1=sinb, op=ALU.mult))
            kb.op('dve', [qf_, cs_], [rt_], lambda en: en.tensor_tensor(out=rt_[:, 3], in0=x2, in1=cosb, op=ALU.mult))
            kb.op('dve', [rt_], [qb_], lambda en: en.tensor_tensor(out=qb_[:, :, 64:80], in0=rt_[:, 0], in1=rt_[:, 1], op=ALU.subtract))
            kb.op('dve', [rt_], [qb_], lambda en: en.tensor_tensor(out=qb_[:, :, 80:96], in0=rt_[:, 2], in1=rt_[:, 3], op=ALU.add))
            kb.op('pool', [qf_], [qb_], lambda en: en.tensor_copy(out=qb_[:, :, 0:64], in_=qf_[:, :, 0:64]))
            for h in range(8):
                kb.op('pe', [qb_, ident], [T1], lambda en, h=h: en.transpose(out=T1[0:96, h, :], in_=qb_[:, h, :], identity=ident[:]))
            MQT_ = MQT[s2]
            kb.op('act', [T1], [MQT_], lambda en: en.activation(out=MQT_[:, :, cols], in_=T1[0:96, :, :], func=AF.Copy,
                                                                scale=96 ** -0.5))
            if lim < 7:
                continue
            for c in range(2):
                kb.op('pe', [cT_, Wukv], [M[c]], lambda en, c=c: en.matmul(
                    M[c][:, 0:512], lhsT=cT_[:, 2, :], rhs=Wukv[:, 0, c * 512:(c + 1) * 512], start=True, stop=True))
            for c in range(2):
                kb.op('act', [M[c]], [sq_], lambda en, c=c: en.activation(
                    out=sq_[:, c * 256:(c + 1) * 256].rearrange('p (h d) -> p h d', h=4),
                    in_=M[c][:, 0:512].rearrange('p (h d) -> p h d', h=4)[:, :, 0:64], func=AF.Square))
            kb.op('dve', [sq_], [ssq_], lambda en: en.tensor_reduce(out=ssq_[:, 1:9], in_=sq_[:, 0:512].rearrange(
                'p (h d) -> p h d', h=8), axis=AX.X, op=ALU.add))
            kb.op('dve', [ssq_], [ssq_], lambda en: en.tensor_scalar(out=ssq_[:, 1:9], in0=ssq_[:, 1:9], scalar1=ssq_[:, 15:16],
                                                                     scalar2=None, op0=ALU.add))
            kb.op('act', [ssq_], [rstd_], lambda en: en.activation(out=rstd_[:, 1:9], in_=ssq_[:, 1:9], func=AF.Sqrt,
                                                                   bias=EPS, scale=1.0 / 96))
            kb.op('dve', [rstd_], [rstd_], lambda en: en.reciprocal(out=rstd_[:, 1:9], in_=rstd_[:, 1:9]))
            kt_ = kt[r]
            kbf_ = kbf[r]
            vb2_ = vb2[r]
            for c in range(2):
                kb.op('dve', [M[c], rstd_], [kt_], lambda en, c=c: en.tensor_tensor(
                    out=kt_[:, c * 4:(c + 1) * 4, :], in0=M[c][:, 0:512].rearrange('p (h d) -> p h d', h=4)[:, :, 0:64],
                    in1=bcast_mid(rstd_[:, 1 + c * 4:5 + c * 4], 64), op=ALU.mult))
                kb.op('act', [M[c]], [vb2_], lambda en, c=c: en.copy(
                    out=vb2_[:, c * 4:(c + 1) * 4, :], in_=M[c][:, 0:512].rearrange('p (h d) -> p h d', h=4)[:, :, 64:128]))
            kb.dma('sp', d['mla_v'][rows, :, :], vb2_[:], [vb2_], [d['mla_v_b']], vb2_)
            kb.op('pool', [kt_, mkg], [kbf_], lambda en: en.tensor_tensor(
                out=kbf_[:, :, 0:64], in0=kt_[:], in1=mkg[:, 0:64].unsqueeze(1).broadcast_to([128, 8, 64]), op=ALU.mult))
            kr2_ = kr2[r]
            kb.op('pool', [krp_, cs_], [kr2_], lambda en: en.tensor_tensor(out=kr2_[:, 0, 0:16], in0=krp_[:, 0:16], in1=cs_[:, 0:16], op=ALU.mult))
            kb.op('pool', [krp_, cs_], [kr2_], lambda en: en.tensor_tensor(out=kr2_[:, 0, 16:32], in0=krp_[:, 0:16], in1=cs_[:, 16:32], op=ALU.mult))
            kb.op('pool', [krp_, cs_], [kr2_], lambda en: en.tensor_tensor(out=kr2_[:, 1, 0:16], in0=krp_[:, 16:32], in1=cs_[:, 16:32], op=ALU.mult))
            kb.op('pool', [krp_, cs_], [kr2_], lambda en: en.tensor_tensor(out=kr2_[:, 1, 16:32], in0=krp_[:, 16:32], in1=cs_[:, 0:16], op=ALU.mult))
            kb.op('pool', [kr2_], [kr2_], lambda en: en.tensor_tensor(out=kr2_[:, 0, 0:16], in0=kr2_[:, 0, 0:16], in1=kr2_[:, 1, 0:16], op=ALU.subtract))
            kb.op('pool', [kr2_], [kr2_], lambda en: en.tensor_tensor(out=kr2_[:, 0, 16:32], in0=kr2_[:, 0, 16:32], in1=kr2_[:, 1, 16:32], op=ALU.add))
            kb.op('dve', [kr2_, rstd_], [kbf_], lambda en: en.tensor_tensor(
                out=kbf_[:, :, 64:96], in0=kr2_[:, 0, :].unsqueeze(1).broadcast_to([128, 8, 32]), in1=bcast_mid(rstd_[:, 1:9], 32), op=ALU.mult))
            for h in range(8):
                kb.op('pe', [kbf_, ident], [T0], lambda en, h=h: en.transpose(out=T0[0:96, h, :], in_=kbf_[:, h, :], identity=ident[:]))
            MKT_ = MKT[s2]
            kb.op('act', [T0], [MKT_], lambda en: en.copy(out=MKT_[:, :, cols], in_=T0[0:96, :, :]))
            if lim < 8:
                continue
            if j == 3:
                sc = slice(st * 512, (st + 1) * 512)
                kb.dma('sp', d['nsa_qT'][:, :, sc], QT_[:], [QT_], [d['nsa_qT_b']], QT_)
                kb.dma('sp', d['nsa_kT'][:, :, sc], KST_[:], [KST_], [d['nsa_kT_b']], KST_)
                kb.dma('sp', d['nsa_cT'][:, :, sc], CT_[:], [CT_], [d['nsa_cT_b']], CT_)
                kb.dma('sp', d['mla_qT'][:, :, sc], MQT_[:], [MQT_], [d['mla_qT_b']], MQT_)
                kb.dma('sp', d['mla_kT'][:, :, sc], MKT_[:], [MKT_], [d['mla_kT_b']], MKT_)


SCRATCH = {
    'nsa_qT': ([64, 8, S], BF16), 'nsa_kT': ([64, 4, S], BF16), 'nsa_cT': ([128, 2, S], BF16),
    'nsa_v': ([S, 4, 64], BF16), 'gates': ([S, 24], F32),
    'mla_qT': ([96, 8, S], BF16), 'mla_kT': ([96, 8, S], BF16), 'mla_v': ([S, 8, 64], BF16),
    'mix': ([S, D], BF16), 'x1a': ([S, D], F32), 'x1h': ([S, D], F32), 'x1': ([S, D], F32),
    'x2a': ([S, D], F32), 'moe_gate': ([S, 8], F32), 'acc0': ([S, D], F32), 'acc1': ([S, D], F32),
    'nsa_kcT': ([64, 2, 256], BF16), 'nsa_vc': ([128, 2, 2, 64], BF16),
}
INPUTS = {
    'x': [S, D], 'c_ident': [128, 128], 'c_cossin': [S, 32],
    'ev_mix_row': [1, D], 'ev_w_in': [D, EVEN_W], 'nsa_q_col': [64, 1], 'nsa_k_col': [64, 3],
    'cqkv_row': [1, 384], 'mla_w_uq': [256, 768], 'mla_w_ukv': [128, 1024],
    'mla_q_row': [1, 768], 'mla_k_row': [1, 96], 'c_mla_mask': [128, 4, 512],
    'ev_w_out': [D, D], 'ev_ffn_row': [1, D], 'ffn_w_gate': [D, D_FF], 'ffn_w_up': [D, D_FF], 'ffn_w_down': [D_FF, D],
    'od_mix_row': [1, D], 'od_w_in': [D, 3 * D], 'conv_col': [128, 8, 3], 'od_w_out': [D, D],
    'od_ffn_row': [1, D], 'moe_wrT': [1, 8 * D], 'moe_b_row': [1, 8],
    'nsa_cmp_w1': [2, 2048, 256], 'nsa_cmp_w2': [2, 256, 64], 'nsa_posT': [2, 64, 32],
    'c_btmask': [128, 4, 128], 'c_ov': [128, 2, 64], 'c_ex': [64, S], 'nsa_bt': [2, 128, 4, 4, 128],
    'nsa_cbias': [NT, 2, 128, 2, 4, 128], 'c_cmask': [NT, 128, 2, 128], 'c_force': [NT, 128, 64],
    'moe_w_gate': [NE, D, DFE], 'moe_w_up': [NE, D, DFE], 'moe_w_down': [NE, DFE, D],
}


def col_layout(v, kc):
    return np.ascontiguousarray(np.asarray(v, np.float32).reshape(kc, 128).T)


def t5_bucket_np(dist):
    n = np.maximum(dist, 0)
    nf = np.maximum(n, 16).astype(np.float32)
    large = 16 + (np.log(nf / np.float32(16)) / np.float32(math.log(8.0)) * np.float32(16)).astype(np.int32)
    return np.where(n < 16, n, np.minimum(large, 31))


def nsa_index_tables():
    k = np.arange(128)[:, None]
    q = np.arange(128)[None, :]
    dists = [q - k, 128 + q - k, np.full((128, 128), 300), np.full((128, 128), 400)]
    bt_idx = np.stack([t5_bucket_np(x) for x in dists], 1)
    bt_mask = np.stack([np.where(q >= k, 0.0, -30000.0), np.zeros((128, 128)), np.zeros((128, 128)),
                        np.where(k > q, 0.0, -30000.0)], 1).astype(np.float32)
    c = (np.arange(2)[None, :, None] * 128 + np.arange(128)[:, None, None])[None]
    t = (np.arange(NT)[:, None, None, None] * 128 + np.arange(128)[None, None, None, :])
    dc = t - (16 * c + 31)
    cb_idx = t5_bucket_np(dc)
    cb_mask = np.where((dc >= 0) & (c < 255), 0.0, -30000.0).astype(np.float32)
    return bt_idx, bt_mask, cb_idx, cb_mask


def nsa_consts():
    _, bt_mask, _, cb_mask = nsa_index_tables()
    c = {'c_btmask': bt_mask, 'c_cmask': cb_mask}
    cc = np.arange(256)[:, None]
    j = np.arange(64)[None, :]
    ov = ((16 * cc < 64 * j + 64) & (16 * cc + 31 >= 64 * j) & (cc < 255)).astype(np.float32)
    c['c_ov'] = np.ascontiguousarray(ov.reshape(2, 128, 64).transpose(1, 0, 2))
    c['c_ex'] = (np.arange(S)[None, :] // 64 == np.arange(64)[:, None]).astype(np.float32)
    t = np.arange(S)[:, None]
    c['c_force'] = np.where((j == t // 64) | (j == 0), 1e9, 0.0).astype(np.float32).reshape(NT, 128, 64)
    return c


def host_consts():
    c = {}
    c['c_ident'] = np.eye(128, dtype=np.float32)
    half = 16
    inv_freq = 10000.0 ** (-np.arange(half, dtype=np.float32) / half)
    ang = np.arange(S, dtype=np.float32)[:, None] * inv_freq[None, :]
    c['c_cossin'] = np.concatenate([np.cos(ang), np.sin(ang)], axis=1).astype(np.float32)
    c.update(nsa_consts())
    kk = np.arange(128)[:, None, None] + 128 * np.arange(4)[None, :, None]
    c['c_mla_mask'] = np.where(np.arange(512)[None, None, :] >= kk, 0.0, -30000.0).astype(np.float32)
    return c


def prep_core_inputs(inp, b, consts):
    f = lambda a: np.ascontiguousarray(np.asarray(a, np.float32))
    m = dict(consts)
    m['x'] = f(inp['x'][b])
    m['ev_mix_row'] = f(np.asarray(inp['ev_mix_norm'][0]).reshape(1, D))
    m['ev_w_in'] = f(inp['ev_w_in'][0])
    m['nsa_q_col'] = f(np.asarray(inp['nsa_q_norm'][0]).reshape(64, 1))
    m['nsa_k_col'] = f(np.asarray(inp['nsa_k_norm'][0]).T)
    m['cqkv_row'] = f(np.concatenate([np.asarray(inp['mla_cq_norm'][0]), np.asarray(inp['mla_ckv_norm'][0])]).reshape(1, 384))
    m['mla_w_uq'] = f(inp['mla_w_uq'][0])
    m['mla_w_ukv'] = f(inp['mla_w_ukv'][0])
    m['mla_q_row'] = f(np.tile(np.asarray(inp['mla_q_norm'][0]), 8).reshape(1, 768))
    m['mla_k_row'] = f(np.asarray(inp['mla_k_norm'][0]).reshape(1, 96))
    m['ev_w_out'] = f(inp['ev_w_out'][0])
    m['nsa_cmp_w1'] = f(inp['nsa_cmp_w1'][0])
    m['nsa_cmp_w2'] = f(inp['nsa_cmp_w2'][0])
    m['nsa_posT'] = f(np.asarray(inp['nsa_cmp_pos'][0]).transpose(0, 2, 1))
    bt_idx, _, cb_idx, _ = nsa_index_tables()
    rb = np.asarray(inp['rel_bias'], np.float32).reshape(32, 2, 4)
    m['nsa_bt'] = f(rb[bt_idx].transpose(3, 0, 1, 4, 2))
    m['nsa_cbias'] = f(rb[cb_idx].transpose(0, 4, 1, 2, 5, 3))
    m['ev_ffn_row'] = f(np.asarray(inp['ev_ffn_norm'][0]).reshape(1, D))
    m['ffn_w_gate'] = f(inp['ffn_w_gate'][0])
    m['ffn_w_up'] = f(inp['ffn_w_up'][0])
    m['ffn_w_down'] = f(inp['ffn_w_down'][0])
    m['od_mix_row'] = f(np.asarray(inp['od_mix_norm'][0]).reshape(1, D))
    m['od_w_in'] = f(inp['od_w_in'][0])
    m['conv_col'] = f(np.asarray(inp['conv_w'][0]).reshape(3, 8, 128).transpose(2, 1, 0))
    m['od_w_out'] = f(inp['od_w_out'][0])
    m['od_ffn_row'] = f(np.asarray(inp['od_ffn_norm'][0]).reshape(1, D))
    m['moe_wrT'] = f(np.asarray(inp['moe_w_router'][0]).T.reshape(1, 8 * D))
    m['moe_b_row'] = f(np.asarray(inp['moe_b_router'][0]).reshape(1, 8))
    m['moe_w_gate'] = f(inp['moe_w_gate'][0])
    m['moe_w_up'] = f(inp['moe_w_up'][0])
    m['moe_w_down'] = f(inp['moe_w_down'][0])
    return m


def phase_mla(kb, d, nqc=8):
    with kb.phase() as ph:
        identf = ph.sb('identf', [128, 128], F32)
        ident = ph.sb('ident', [128, 128], BF16)
        kb.dma('sp', identf[:], d['c_ident'][:, :], [], [identf], identf)
        kb.op('dve', [identf], [ident], lambda en: en.tensor_copy(out=ident[:], in_=identf[:]))
        stg = ph.sb('mstg', [128, 4, 512], F32)
        Mm = ph.sb('Mm', [128, 4, 512], BF16)
        kb.dma('sp', stg[:], d['c_mla_mask'][:, :, :], [], [stg], stg)
        kb.op('dve', [stg], [Mm], lambda en: en.tensor_copy(out=Mm[:], in_=stg[:]))
        MK = ph.sb('MK', [128, 8, S], BF16)
        kb.op('dve', [], [MK], lambda en: en.memset(MK[96:128], 0.0))
        for h in range(8):
            kb.dma(('sp', 'act')[h % 2], MK[0:96, h, :], d['mla_kT'][:, h, :], [d['mla_kT_b']], [MK], MK)
        V = ph.sb('V', [128, NT, 8, 65], BF16)
        kb.op('pool', [], [V], lambda en: en.memset(V[:], 1.0))
        for kt in range(NT):
            kb.dma(('sp', 'act')[kt % 2], V[:, kt, :, 0:64], d['mla_v'][kt * 128:(kt + 1) * 128, :, :], [d['mla_v_b']], [V], V)
        MQ = ring(ph, 'MQ', [128, 8, 512], BF16)
        for b in MQ:
            kb.op('pool', [], [b], lambda en, b=b: en.memset(b[96:128], 0.0))
        Sr = [ph.ps('S%d' % i, [128, 512], F32) for i in range(2)]
        Or = [ph.ps('O%d' % i, [128, 65], F32) for i in range(4)]
        Pr = ring(ph, 'P', [128, 512], BF16, 3)
        om = ring(ph, 'om', [128, 4, 512], BF16)
        rden = ring(ph, 'rden', [128, 4], F32)
        it = 0
        for qc in range(nqc):
            MQ_ = MQ[qc % 2]
            kb.dma('sp', MQ_[0:96], d['mla_qT'][:, :, qc * 512:(qc + 1) * 512], [d['mla_qT_b']], [MQ_], MQ_)
            om_ = om[qc % 2]
            for h in range(8):
                nk = 4 * qc + 4
                slots = {}

                def emit_s(kt):
                    nonlocal it
                    S_, P_ = Sr[it % 2], Pr[it % 3]
                    it += 1
                    slots[kt] = (S_, P_)
                    c0 = max(0, kt - 4 * qc) * 128
                    diag = kt >= 4 * qc
                    kb.op('pe', [MK, MQ_], [S_], lambda en: en.matmul(S_[:, c0:512], lhsT=MK[:, h, kt * 128:(kt + 1) * 128],
                                                                     rhs=MQ_[:, h, c0:512], start=True, stop=not diag))
                    if diag:
                        kb.op('pe', [ident, Mm], [S_], lambda en: en.matmul(S_[:, c0:512], lhsT=ident[:], rhs=Mm[:, kt - 4 * qc, c0:512],
                                                                           start=False, stop=True))
                emit_s(0)
                for kt in range(nk):
                    if kt + 1 < nk:
                        emit_s(kt + 1)
                    S_, P_ = slots[kt]
                    jmin = max(0, kt - 4 * qc)
                    c0 = jmin * 128
                    kb.op('act', [S_], [P_], lambda en: en.activation(out=P_[:, c0:512], in_=S_[:, c0:512], func=AF.Exp))
                    for j in range(jmin, 4):
                        kb.op('pe', [P_, V], [Or[j]], lambda en, j=j: en.matmul(Or[j][:, :], lhsT=P_[:, j * 128:(j + 1) * 128],
                                                                               rhs=V[:, kt, h, :], start=(kt == 0), stop=(kt == 4 * qc + j)))
                rd = rden[h % 2]
                for j in range(4):
                    kb.op('dve', [Or[j]], [rd], lambda en, j=j: en.reciprocal(out=rd[:, j:j + 1], in_=Or[j][:, 64:65]))
                    kb.op('dve', [Or[j], rd], [om_], lambda en, j=j: en.tensor_scalar(out=om_[:, j, h * 64:(h + 1) * 64], in0=Or[j][:, 0:64],
                                                                                     scalar1=rd[:, j:j + 1], scalar2=None, op0=ALU.mult))
            kb.dma('sp', d['mix'][qc * 512:(qc + 1) * 512, 512:1024].rearrange('(j p) c -> p j c', p=128), om_[:],
                   [om_], [d['mix_b']], om_)


def make_ident(kb, ph, d):
    identf = ph.sb('identf', [128, 128], F32)
    ident = ph.sb('ident', [128, 128], BF16)
    kb.dma('sp', identf[:], d['c_ident'][:, :], [], [identf], identf)
    kb.op('dve', [identf], [ident], lambda en: en.tensor_copy(out=ident[:], in_=identf[:]))
    return ident


def norm_transpose(kb, x_, ssq_, rstd_, junk, xs_, T0, ident, dstT, cols):
    kb.op('pool', [], [ssq_], lambda en: en.memset(ssq_[:, 0:1], 0.0))
    kb.op('act', [x_], [junk, ssq_], lambda en: en.activation(out=junk[:], in_=x_[:], func=AF.Square, accum_out=ssq_[:, 0:1]))
    kb.op('act', [ssq_], [rstd_], lambda en: en.activation(out=rstd_[:, 0:1], in_=ssq_[:, 0:1], func=AF.Sqrt, bias=EPS, scale=1.0 / D))
    kb.op('dve', [rstd_], [rstd_], lambda en: en.reciprocal(out=rstd_[:, 0:1], in_=rstd_[:, 0:1]))
    kb.op('dve', [x_, rstd_], [xs_], lambda en: en.tensor_scalar(out=xs_[:], in0=x_[:], scalar1=rstd_[:, 0:1], scalar2=None, op0=ALU.mult))
    for k in range(8):
        kb.op('pe', [xs_, ident], [T0], lambda en, k=k: en.transpose(out=T0[:, k, :], in_=xs_[:, k * 128:(k + 1) * 128], identity=ident[:]))
    kb.op('act', [T0], [dstT], lambda en: en.copy(out=dstT[:, 0:8, cols], in_=T0[:]))


def phase_wout(kb, d, nt=NT):
    with kb.phase() as ph:
        ident = make_ident(kb, ph, d)
        Wo = ph.sb('Wo', [128, 8, D], BF16)
        load_w(kb, ph, Wo, d['ev_w_out'], D, D)
        T0 = ph.ps('T0', [128, 8, 128], BF16)
        M = [ph.ps('M%d' % i, [128, 512], F32) for i in range(4)]
        mx = ring(ph, 'mx', [128, D], BF16)
        xr = ring(ph, 'xr', [128, D], F32)
        mT = ring(ph, 'mT', [128, 8, 128], BF16)
        xo = ring(ph, 'xo', [128, D], F32)
        for tt in range(nt):
            r = tt % 2
            rows = slice(tt * 128, (tt + 1) * 128)
            kb.dma('sp', mx[r][:], d['mix'][rows, :], [d['mix_b']], [mx[r]], mx[r])
            kb.dma('act', xr[r][:], d['x'][rows, :], [], [xr[r]], xr[r])
            for k in range(8):
                kb.op('pe', [mx[r], ident], [T0], lambda en, k=k: en.transpose(out=T0[:, k, :], in_=mx[r][:, k * 128:(k + 1) * 128], identity=ident[:]))
            kb.op('act', [T0], [mT[r]], lambda en: en.copy(out=mT[r][:], in_=T0[:]))
            for c in range(2):
                Mc = M[(tt * 2 + c) % 4]
                for k in range(8):
                    kb.op('pe', [mT[r], Wo], [Mc], lambda en, k=k, c=c: en.matmul(Mc[:, :], lhsT=mT[r][:, k, :], rhs=Wo[:, k, c * 512:(c + 1) * 512],
                                                                                 start=(k == 0), stop=(k == 7)))
                kb.op('dve', [Mc, xr[r]], [xo[r]], lambda en, c=c: en.tensor_tensor(out=xo[r][:, c * 512:(c + 1) * 512], in0=Mc[:, :],
                                                                                   in1=xr[r][:, c * 512:(c + 1) * 512], op=ALU.add))
            kb.dma('sp', d['x1a'][rows, :], xo[r][:], [xo[r]], [d['x1a_b']], xo[r])


def load_w_thunks(kb, dst, src, K, N):
    return [lambda kc=kc: kb.dma('pool', dst[:, kc, :], src[kc * 128:(kc + 1) * 128, :], [], [dst], dst) for kc in range(K // 128)]


def expert_passes(kb, d, passes, nst=8):
    with kb.phase() as ph:
        ident = make_ident(kb, ph, d)
        WS = [dict(Wg=ph.sb('Wg%d' % i, [128, 8, DFE], BF16), Wu=ph.sb('Wu%d' % i, [128, 8, DFE], BF16),
                   Wd=ph.sb('Wd%d' % i, [128, 11, D], BF16)) for i in range(2)]
        grow = ph.sb('grow', [128, D], F32)
        kb.dma('sp', grow[:], bc_row(passes[0]['grow'], D), [], [grow], grow)
        T0 = ph.ps('T0', [128, 8, 128], BF16)
        G = [ph.ps('G%d' % i, [128, 512], F32) for i in range(2)]
        U = [ph.ps('U%d' % i, [128, 512], F32) for i in range(2)]
        Y = [ph.ps('Y%d' % i, [128, 512], F32) for i in range(2)]
        xr = ring(ph, 'xr', [128, D], F32)
        rs = ring(ph, 'rs', [128, D], F32)
        gt = ring(ph, 'gt', [128, 8], F32)
        junk = ph.sb('junk', [128, D], BF16)
        ssq = ring(ph, 'ssq', [128, 1], F32)
        rstd = ring(ph, 'rstd', [128, 1], F32)
        xs = ring(ph, 'xs', [128, D], BF16)
        xnT = ring(ph, 'xnT', [128, 8, 512], BF16)
        sg = ring(ph, 'sg', [128, 512], F32)
        hT = ph.sb('hT', [128, 11, 512], BF16)
        ctr = [0]

        def weight_thunks(p, ws):
            th = load_w_thunks(kb, ws['Wg'], p['wg'], D, DFE)
            th += load_w_thunks(kb, ws['Wu'], p['wu'], D, DFE)
            th += load_w_thunks(kb, ws['Wd'], p['wd'], DFE, D)
            return th

        for t in weight_thunks(passes[0], WS[0]):
            t()
        it = 0
        for pi, p in enumerate(passes):
            ws = WS[pi % 2]
            Wg, Wu, Wd = ws['Wg'], ws['Wu'], ws['Wd']
            pending = weight_thunks(passes[pi + 1], WS[(pi + 1) % 2]) if pi + 1 < len(passes) else []
            per_st = (len(pending) + nst - 1) // nst
            src, resid, dst = d[p['src']], d[p['resid']], d[p['dst']]
            srcb = [d[p['src'] + '_b']] if p['src'] + '_b' in d else []
            resb = [d[p['resid'] + '_b']] if p['resid'] + '_b' in d else []

            def prep_a(st, j):
                tt = st * 4 + j
                r = tt % 2
                rows = slice(tt * 128, (tt + 1) * 128)
                x_, ssq_, rstd_, xs_ = xr[r], ssq[r], rstd[r], xs[r]
                kb.dma('sp', x_[:], src[rows, :], srcb, [x_], x_)
                kb.op('pool', [], [ssq_], lambda en: en.memset(ssq_[:, 0:1], 0.0))
                kb.op('act', [x_], [junk, ssq_], lambda en: en.activation(out=junk[:], in_=x_[:], func=AF.Square, accum_out=ssq_[:, 0:1]))
                kb.op('act', [ssq_], [rstd_], lambda en: en.activation(out=rstd_[:, 0:1], in_=ssq_[:, 0:1], func=AF.Sqrt, bias=EPS, scale=1.0 / D))
                kb.op('dve', [rstd_], [rstd_], lambda en: en.reciprocal(out=rstd_[:, 0:1], in_=rstd_[:, 0:1]))
                kb.op('dve', [x_, rstd_, grow], [xs_], lambda en: en.scalar_tensor_tensor(out=xs_[:], in0=x_[:], scalar=rstd_[:, 0:1], in1=grow[:],
                                                                                         op0=ALU.mult, op1=ALU.mult))

            def prep_b(st, j):
                tt = st * 4 + j
                xs_ = xs[tt % 2]
                xnT_ = xnT[st % 2]
                for k in range(8):
                    kb.op('pe', [xs_, ident], [T0], lambda en, k=k: en.transpose(out=T0[:, k, :], in_=xs_[:, k * 128:(k + 1) * 128], identity=ident[:]))
                kb.op('act', [T0], [xnT_], lambda en: en.copy(out=xnT_[:, 0:8, j * 128:(j + 1) * 128], in_=T0[:]))

            for j in range(4):
                prep_a(0, j)
                prep_b(0, j)
            for st in range(nst):
                xnT_ = xnT[st % 2]
                nxt = st + 1 < nst
                for f in range(11):
                    G_, U_ = G[f % 2], U[f % 2]
                    for k in range(8):
                        kb.op('pe', [Wg, xnT_], [G_], lambda en, k=k: en.matmul(G_[:, :], lhsT=Wg[:, k, f * 128:(f + 1) * 128], rhs=xnT_[:, k, :],
                                                                               start=(k == 0), stop=(k == 7)))
                    for k in range(8):
                        kb.op('pe', [Wu, xnT_], [U_], lambda en, k=k: en.matmul(U_[:, :], lhsT=Wu[:, k, f * 128:(f + 1) * 128], rhs=xnT_[:, k, :],
                                                                               start=(k == 0), stop=(k == 7)))
                    sg_ = sg[f % 2]
                    kb.op('act', [G_], [sg_], lambda en: en.activation(out=sg_[:], in_=G_[:, :], func=AF.Silu))
                    kb.op('dve', [sg_, U_], [hT], lambda en: en.tensor_tensor(out=hT[:, f, :], in0=U_[:, :], in1=sg_[:], op=ALU.mult))
                    if nxt and f in (2, 4, 6, 8):
                        prep_b(st + 1, f // 2 - 1)
                    if nxt and f in (0, 2, 4, 6):
                        prep_a(st + 1, f // 2)
                for j in range(4):
                    tt = st * 4 + j
                    r = tt % 2
                    rows = slice(tt * 128, (tt + 1) * 128)
                    kb.dma('sp', rs[r][:], resid[rows, :], resb, [rs[r]], rs[r])
                    if p['gate'] is not None:
                        kb.dma('sp', gt[r][:], d[p['gate'][0]][rows, :], [d[p['gate'][0] + '_b']], [gt[r]], gt[r])
                    for c in range(2):
                        Y_ = Y[it % 2]
                        it += 1
                        for f in range(11):
                            kb.op('pe', [hT, Wd], [Y_], lambda en, f=f, c=c: en.matmul(Y_[:, :], lhsT=hT[:, f, j * 128:(j + 1) * 128],
                                                                                      rhs=Wd[:, f, c * 512:(c + 1) * 512], start=(f == 0), stop=(f == 10)))
                        cs_ = slice(c * 512, (c + 1) * 512)
                        if p['gate'] is not None:
                            e = p['gate'][1]
                            kb.op('dve', [Y_, rs[r], gt[r]], [rs[r]], lambda en: en.scalar_tensor_tensor(
                                out=rs[r][:, cs_], in0=Y_[:, :], scalar=gt[r][:, e:e + 1], in1=rs[r][:, cs_], op0=ALU.mult, op1=ALU.add))
                        else:
                            kb.op('dve', [Y_, rs[r]], [rs[r]], lambda en: en.tensor_tensor(out=rs[r][:, cs_], in0=Y_[:, :], in1=rs[r][:, cs_], op=ALU.add))
                    kb.dma('sp', dst[rows, :], rs[r][:], [rs[r]], [d[p['dst'] + '_b']], rs[r])
                for t in pending[st * per_st:(st + 1) * per_st]:
                    t()


def phase_odd_mixer(kb, d, nst=8, fuse_router=True):
    with kb.phase() as ph:
        ident = make_ident(kb, ph, d)
        Wi = ph.sb('Wi', [128, 8, 3 * D], BF16)
        Wo = ph.sb('Wo', [128, 8, D], BF16)
        cw = ph.sb('cw', [128, 8, 3], F32)
        grow = ph.sb('grow', [128, D], F32)
        kb.dma('sp', grow[:], bc_row(d['od_mix_row'], D), [], [grow], grow)
        kb.dma('sp', cw[:], d['conv_col'][:, :, :], [], [cw], cw)
        load_w(kb, ph, Wi, d['od_w_in'], D, 3 * D)
        load_w(kb, ph, Wo, d['od_w_out'], D, D)
        carry = ph.sb('carry', [128, 8, 2], F32)
        kb.op('pool', [], [carry], lambda en: en.memset(carry[:], 0.0))
        if fuse_router:
            wr = ph.sb('wr', [128, 8, D], F32)
            grow2 = ph.sb('grow2', [128, D], F32)
            brow = ph.sb('brow', [128, 8], F32)
            kb.dma('sp', wr[:], bass.AP(d['moe_wrT'], 0, [[0, 128], [1, 8 * D]]), [], [wr], wr)
            kb.dma('sp', grow2[:], bc_row(d['od_ffn_row'], D), [], [grow2], grow2)
            kb.dma('sp', brow[:], bc_row(d['moe_b_row'], 8), [], [brow], brow)
            kb.op('dve', [wr, grow2], [wr], lambda en: en.tensor_tensor(out=wr[:], in0=wr[:], in1=grow2[:].unsqueeze(1).broadcast_to([128, 8, D]), op=ALU.mult))
            xn2 = ring(ph, 'xn2', [128, D], F32)
            jk2 = ring(ph, 'jk2', [128, D], BF16)
            ssq2 = ring(ph, 'ssq2', [128, 1], F32)
            rstd2 = ring(ph, 'rstd2', [128, 1], F32)
            lg = ring(ph, 'lg', [128, 8], F32)
            m8 = ring(ph, 'm8', [128, 8], F32)
            ww = ring(ph, 'ww', [128, 4], F32)
            g1 = ring(ph, 'g1', [128, 8], F32)
            g2 = ring(ph, 'g2', [128, 8], F32)
        T0 = ph.ps('T0', [128, 8, 128], BF16)
        Bp = [ph.ps('Bp%d' % i, [128, 512], F32) for i in range(2)]
        Cp = ph.ps('Cp', [128, 512], F32)
        Up = ph.ps('Up', [128, 512], F32)
        Y = [ph.ps('Y%d' % i, [128, 512], F32) for i in range(2)]
        xr = ring(ph, 'xr', [128, D], F32)
        junk = ph.sb('junk', [128, D], BF16)
        ssq = ring(ph, 'ssq', [128, 1], F32)
        rstd = ring(ph, 'rstd', [128, 1], F32)
        xs = ring(ph, 'xs', [128, D], BF16)
        xnT = ring(ph, 'xnT', [128, 8, 512], BF16)
        csb = ring(ph, 'csb', [128, 512], F32)
        cu = ring(ph, 'cu', [128, 514], F32)
        yy = ring(ph, 'yy', [128, 512], F32)
        mT = ring(ph, 'mT', [128, 8, 512], BF16)
        xo = ring(ph, 'xo', [128, D], F32)
        def prep_a(st, j):
            tt = st * 4 + j
            r = tt % 2
            rows = slice(tt * 128, (tt + 1) * 128)
            x_ = xr[r]
            kb.dma('sp', x_[:], d['x1'][rows, :], [d['x1_b']], [x_], x_)
            kb.op('pool', [], [ssq[r]], lambda en: en.memset(ssq[r][:, 0:1], 0.0))
            kb.op('act', [x_], [junk, ssq[r]], lambda en: en.activation(out=junk[:], in_=x_[:], func=AF.Square, accum_out=ssq[r][:, 0:1]))
            kb.op('act', [ssq[r]], [rstd[r]], lambda en: en.activation(out=rstd[r][:, 0:1], in_=ssq[r][:, 0:1], func=AF.Sqrt, bias=EPS, scale=1.0 / D))
            kb.op('dve', [rstd[r]], [rstd[r]], lambda en: en.reciprocal(out=rstd[r][:, 0:1], in_=rstd[r][:, 0:1]))
            kb.op('dve', [x_, rstd[r], grow], [xs[r]], lambda en: en.scalar_tensor_tensor(out=xs[r][:], in0=x_[:], scalar=rstd[r][:, 0:1], in1=grow[:],
                                                                                        op0=ALU.mult, op1=ALU.mult))

        def prep_b(st, j):
            tt = st * 4 + j
            r = tt % 2
            xnT_ = xnT[st % 2]
            for k in range(8):
                kb.op('pe', [xs[r], ident], [T0], lambda en, k=k: en.transpose(out=T0[:, k, :], in_=xs[r][:, k * 128:(k + 1) * 128], identity=ident[:]))
            kb.op('act', [T0], [xnT_], lambda en: en.copy(out=xnT_[:, :, j * 128:(j + 1) * 128], in_=T0[:]))

        it = 0
        for st in range(nst):
            xnT_, mT_ = xnT[st % 2], mT[st % 2]
            if st == 0:
                for j in range(4):
                    prep_a(0, j)
                    prep_b(0, j)
            nxt = st + 1 < nst
            for cc in range(8):
                Bp_ = Bp[cc % 2]
                for (P_, off) in ((Bp_, 0), (Cp, D), (Up, 2 * D)):
                    for k in range(8):
                        kb.op('pe', [Wi, xnT_], [P_], lambda en, k=k: en.matmul(P_[:, :], lhsT=Wi[:, k, off + cc * 128:off + (cc + 1) * 128], rhs=xnT_[:, k, :],
                                                                               start=(k == 0), stop=(k == 7)))
                csb_, cu_, yy_ = csb[cc % 2], cu[cc % 2], yy[cc % 2]
                kb.op('act', [Cp], [csb_], lambda en: en.copy(out=csb_[:], in_=Cp[:, :]))
                kb.op('dve', [csb_, Up], [cu_], lambda en: en.tensor_tensor(out=cu_[:, 2:514], in0=Up[:, :], in1=csb_[:], op=ALU.mult))
                kb.op('pool', [carry], [cu_], lambda en: en.tensor_copy(out=cu_[:, 0:2], in_=carry[:, cc, :]))
                kb.op('pool', [cu_], [carry], lambda en: en.tensor_copy(out=carry[:, cc, :], in_=cu_[:, 512:514]))
                kb.op('dve', [cu_, cw], [yy_], lambda en: en.tensor_scalar(out=yy_[:], in0=cu_[:, 0:512], scalar1=cw[:, cc, 0:1], scalar2=None, op0=ALU.mult))
                kb.op('dve', [cu_, cw, yy_], [yy_], lambda en: en.scalar_tensor_tensor(out=yy_[:], in0=cu_[:, 1:513], scalar=cw[:, cc, 1:2], in1=yy_[:],
                                                                                     op0=ALU.mult, op1=ALU.add))
                kb.op('dve', [cu_, cw, yy_], [yy_], lambda en: en.scalar_tensor_tensor(out=yy_[:], in0=cu_[:, 2:514], scalar=cw[:, cc, 2:3], in1=yy_[:],
                                                                                     op0=ALU.mult, op1=ALU.add))
                kb.op('dve', [yy_, Bp_], [mT_], lambda en: en.tensor_tensor(out=mT_[:, cc, :], in0=Bp_[:, :], in1=yy_[:], op=ALU.mult))
                if nxt and cc in (1, 3, 5, 7):
                    prep_b(st + 1, (cc - 1) // 2)
                if nxt and cc in (0, 2, 4, 6):
                    prep_a(st + 1, cc // 2)
            for j in range(4):
                tt = st * 4 + j
                r = tt % 2
                rows = slice(tt * 128, (tt + 1) * 128)
                kb.dma('sp', xo[r][:], d['x1'][rows, :], [d['x1_b']], [xo[r]], xo[r])
                for c in range(2):
                    Y_ = Y[it % 2]
                    it += 1
                    for cc in range(8):
                        kb.op('pe', [mT_, Wo], [Y_], lambda en, cc=cc: en.matmul(Y_[:, :], lhsT=mT_[:, cc, j * 128:(j + 1) * 128], rhs=Wo[:, cc, c * 512:(c + 1) * 512],
                                                                                start=(cc == 0), stop=(cc == 7)))
                    kb.op('dve', [Y_, xo[r]], [xo[r]], lambda en: en.tensor_tensor(out=xo[r][:, c * 512:(c + 1) * 512], in0=Y_[:, :],
                                                                                  in1=xo[r][:, c * 512:(c + 1) * 512], op=ALU.add))
                kb.dma('sp', d['x2a'][rows, :], xo[r][:], [xo[r]], [d['x2a_b']], xo[r])
                if fuse_router:
                    x_, lg_, m8_, ww_ = xo[r], lg[r], m8[r], ww[r]
                    kb.op('pool', [], [ssq2[r]], lambda en: en.memset(ssq2[r][:, 0:1], 0.0))
                    kb.op('pool', [], [lg_], lambda en: en.memset(lg_[:], 0.0))
                    kb.op('act', [x_], [jk2[0], ssq2[r]], lambda en: en.activation(out=jk2[0][:], in_=x_[:], func=AF.Square, accum_out=ssq2[r][:, 0:1]))
                    kb.op('act', [ssq2[r]], [rstd2[r]], lambda en: en.activation(out=rstd2[r][:, 0:1], in_=ssq2[r][:, 0:1], func=AF.Sqrt, bias=EPS, scale=1.0 / D))
                    kb.op('dve', [rstd2[r]], [rstd2[r]], lambda en: en.reciprocal(out=rstd2[r][:, 0:1], in_=rstd2[r][:, 0:1]))
                    kb.op('act', [x_, rstd2[r]], [xn2[r]], lambda en: en.activation(out=xn2[r][:], in_=x_[:], func=AF.Copy, scale=rstd2[r][:, 0:1]))
                    for e in range(8):
                        jk = jk2[e % 2]
                        kb.op('dve', [xn2[r], wr, lg_], [jk, lg_], lambda en: en.scalar_tensor_tensor(out=jk[:], in0=xn2[r][:], scalar=1.0, in1=wr[:, e, :],
                                                                                                    op0=ALU.mult, op1=ALU.mult, accum_out=lg_[:, e:e + 1]))
                    kb.op('dve', [lg_, brow], [lg_], lambda en: en.tensor_tensor(out=lg_[:], in0=lg_[:], in1=brow[:], op=ALU.add))
                    kb.op('dve', [lg_], [m8_], lambda en: en.max(out=m8_[:], in_=lg_[:]))
                    kb.op('dve', [m8_], [ww_], lambda en: en.tensor_tensor(out=ww_[:, 0:1], in0=m8_[:, 1:2], in1=m8_[:, 0:1], op=ALU.subtract))
                    kb.op('act', [ww_], [ww_], lambda en: en.activation(out=ww_[:, 1:2], in_=ww_[:, 0:1], func=AF.Exp))
                    kb.op('dve', [ww_], [ww_], lambda en: en.tensor_scalar(out=ww_[:, 2:3], in0=ww_[:, 1:2], scalar1=1.0, scalar2=None, op0=ALU.add))
                    kb.op('dve', [ww_], [ww_], lambda en: en.reciprocal(out=ww_[:, 2:3], in_=ww_[:, 2:3]))
                    kb.op('dve', [ww_], [ww_], lambda en: en.tensor_tensor(out=ww_[:, 3:4], in0=ww_[:, 1:2], in1=ww_[:, 2:3], op=ALU.mult))
                    kb.op('dve', [lg_, m8_, ww_], [g1[r]], lambda en: en.tensor_scalar(out=g1[r][:], in0=lg_[:], scalar1=m8_[:, 0:1], scalar2=ww_[:, 2:3],
                                                                                      op0=ALU.is_equal, op1=ALU.mult))
                    kb.op('dve', [lg_, m8_, ww_], [g2[r]], lambda en: en.tensor_scalar(out=g2[r][:], in0=lg_[:], scalar1=m8_[:, 1:2], scalar2=ww_[:, 3:4],
                                                                                      op0=ALU.is_equal, op1=ALU.mult))
                    kb.op('dve', [g1[r], g2[r]], [g1[r]], lambda en: en.tensor_tensor(out=g1[r][:], in0=g1[r][:], in1=g2[r][:], op=ALU.add))
                    kb.dma('sp', d['moe_gate'][rows, :], g1[r][:], [g1[r]], [d['moe_gate_b']], g1[r])


def phase_router(kb, d, nt=NT):
    with kb.phase() as ph:
        wr = ph.sb('wr', [128, 8, D], F32)
        grow = ph.sb('grow', [128, D], F32)
        brow = ph.sb('brow', [128, 8], F32)
        kb.dma('sp', wr[:], bass.AP(d['moe_wrT'], 0, [[0, 128], [1, 8 * D]]), [], [wr], wr)
        kb.dma('sp', grow[:], bc_row(d['od_ffn_row'], D), [], [grow], grow)
        kb.dma('sp', brow[:], bc_row(d['moe_b_row'], 8), [], [brow], brow)
        kb.op('dve', [wr, grow], [wr], lambda en: en.tensor_tensor(out=wr[:], in0=wr[:], in1=grow[:].unsqueeze(1).broadcast_to([128, 8, D]), op=ALU.mult))
        xr = ring(ph, 'xr', [128, D], F32)
        xn = ring(ph, 'xn', [128, D], F32)
        junk = ring(ph, 'junk', [128, D], F32)
        ssq = ring(ph, 'ssq', [128, 1], F32)
        rstd = ring(ph, 'rstd', [128, 1], F32)
        lg = ring(ph, 'lg', [128, 8], F32)
        m8 = ring(ph, 'm8', [128, 8], F32)
        ww = ring(ph, 'ww', [128, 4], F32)
        g1 = ring(ph, 'g1', [128, 8], F32)
        g2 = ring(ph, 'g2', [128, 8], F32)
        for tt in range(nt):
            r = tt % 2
            rows = slice(tt * 128, (tt + 1) * 128)
            x_, lg_, m8_, ww_ = xr[r], lg[r], m8[r], ww[r]
            kb.dma('sp', x_[:], d['x2a'][rows, :], [d['x2a_b']], [x_], x_)
            kb.op('pool', [], [ssq[r]], lambda en: en.memset(ssq[r][:, 0:1], 0.0))
            kb.op('act', [x_], [junk[0], ssq[r]], lambda en: en.activation(out=junk[0][:], in_=x_[:], func=AF.Square, accum_out=ssq[r][:, 0:1]))
            kb.op('act', [ssq[r]], [rstd[r]], lambda en: en.activation(out=rstd[r][:, 0:1], in_=ssq[r][:, 0:1], func=AF.Sqrt, bias=EPS, scale=1.0 / D))
            kb.op('dve', [rstd[r]], [rstd[r]], lambda en: en.reciprocal(out=rstd[r][:, 0:1], in_=rstd[r][:, 0:1]))
            kb.op('act', [x_, rstd[r]], [xn[r]], lambda en: en.activation(out=xn[r][:], in_=x_[:], func=AF.Copy, scale=rstd[r][:, 0:1]))
            kb.op('pool', [], [lg_], lambda en: en.memset(lg_[:], 0.0))
            for e in range(8):
                jk = junk[e % 2]
                kb.op('dve', [xn[r], wr, lg_], [jk, lg_], lambda en: en.scalar_tensor_tensor(out=jk[:], in0=xn[r][:], scalar=1.0, in1=wr[:, e, :],
                                                                                             op0=ALU.mult, op1=ALU.mult, accum_out=lg_[:, e:e + 1]))
            kb.op('dve', [lg_, brow], [lg_], lambda en: en.tensor_tensor(out=lg_[:], in0=lg_[:], in1=brow[:], op=ALU.add))
            kb.op('dve', [lg_], [m8_], lambda en: en.max(out=m8_[:], in_=lg_[:]))
            kb.op('dve', [m8_], [ww_], lambda en: en.tensor_tensor(out=ww_[:, 0:1], in0=m8_[:, 1:2], in1=m8_[:, 0:1], op=ALU.subtract))
            kb.op('act', [ww_], [ww_], lambda en: en.activation(out=ww_[:, 1:2], in_=ww_[:, 0:1], func=AF.Exp))
            kb.op('dve', [ww_], [ww_], lambda en: en.tensor_scalar(out=ww_[:, 2:3], in0=ww_[:, 1:2], scalar1=1.0, scalar2=None, op0=ALU.add))
            kb.op('dve', [ww_], [ww_], lambda en: en.reciprocal(out=ww_[:, 2:3], in_=ww_[:, 2:3]))
            kb.op('dve', [ww_], [ww_], lambda en: en.tensor_tensor(out=ww_[:, 3:4], in0=ww_[:, 1:2], in1=ww_[:, 2:3], op=ALU.mult))
            kb.op('dve', [lg_, m8_, ww_], [g1[r]], lambda en: en.tensor_scalar(out=g1[r][:], in0=lg_[:], scalar1=m8_[:, 0:1], scalar2=ww_[:, 2:3],
                                                                              op0=ALU.is_equal, op1=ALU.mult))
            kb.op('dve', [lg_, m8_, ww_], [g2[r]], lambda en: en.tensor_scalar(out=g2[r][:], in0=lg_[:], scalar1=m8_[:, 1:2], scalar2=ww_[:, 3:4],
                                                                              op0=ALU.is_equal, op1=ALU.mult))
            kb.op('dve', [g1[r], g2[r]], [g1[r]], lambda en: en.tensor_tensor(out=g1[r][:], in0=g1[r][:], in1=g2[r][:], op=ALU.add))
            kb.dma('sp', d['moe_gate'][rows, :], g1[r][:], [g1[r]], [d['moe_gate_b']], g1[r])


def phase_cmp(kb, d):
    with kb.phase() as ph:
        ident = make_ident(kb, ph, d)
        kcol = ph.sb('kcol', [64, 3], F32)
        kb.dma('sp', kcol[:], d['nsa_k_col'][:, :], [], [kcol], kcol)
        stg = ph.sb('w1s', [128, 32, 256], F32)
        W1 = ph.sb('W1', [128, 32, 256], BF16)
        w2s = ph.sb('w2s', [128, 2, 64], F32)
        W2 = ph.sb('W2', [128, 2, 64], BF16)
        pTs = ph.sb('pTs', [64, 32], F32)
        pT = ph.sb('pT', [64, 32], BF16)
        cT = ph.sb('cT', [128, S], BF16)
        b1 = ph.sb('b1', [128, 2], F32)
        g1T = ph.sb('g1T', [128, 2, 256], BF16)
        xx = ring(ph, 'xx', [128, 255], F32)
        x2 = ring(ph, 'x2', [128, 255], F32)
        th = ring(ph, 'th', [128, 255], F32)
        kcT = ph.sb('kcT', [64, 2, 256], BF16)
        vc = ph.sb('vc', [128, 2, 2, 64], BF16)
        kn = ring(ph, 'kn', [128, 64], BF16)
        junk = ph.sb('junk', [128, 64], F32)
        ssq = ring(ph, 'ssq', [128, 1], F32)
        rstd = ring(ph, 'rstd', [128, 1], F32)
        kb.op('pool', [], [kcT], lambda en: en.memset(kcT[:], 0.0))
        kb.op('pool', [], [vc], lambda en: en.memset(vc[:], 0.0))
        kb.op('pool', [], [g1T], lambda en: en.memset(g1T[:], 0.0))
        H1 = [ph.ps('H%d' % i, [128, 256], F32) for i in range(2)]
        B1 = ph.ps('B1', [128, 2], F32)
        O2 = [ph.ps('O2%d' % i, [128, 64], F32) for i in range(2)]
        T0 = ph.ps('T0', [128, 8, 128], BF16)
        for kv in range(2):
            w1v = d['nsa_cmp_w1'][kv].rearrange('(l dd) h -> dd l h', dd=64)
            kb.dma('sp', stg[0:64], w1v, [], [stg], stg)
            kb.dma('act', stg[64:128], w1v, [], [stg], stg)
            kb.op('dve', [stg], [W1], lambda en: en.tensor_copy(out=W1[:], in_=stg[:]))
            kb.dma('sp', w2s[:], d['nsa_cmp_w2'][kv].rearrange('(c p) o -> p c o', p=128), [], [w2s], w2s)
            kb.op('dve', [w2s], [W2], lambda en: en.tensor_copy(out=W2[:], in_=w2s[:]))
            kb.dma('sp', pTs[:], d['nsa_posT'][kv], [], [pTs], pTs)
            kb.op('dve', [pTs], [pT], lambda en: en.tensor_copy(out=pT[:], in_=pTs[:]))
            kb.dma('sp', cT[:], d['nsa_cT'][:, kv, :], [d['nsa_cT_b']], [cT], cT)
            for hc in range(2):
                for l in range(32):
                    kb.op('pe', [W1, pT], [B1], lambda en, l=l: en.matmul(B1[:, hc:hc + 1], lhsT=W1[0:64, l, hc * 128:(hc + 1) * 128], rhs=pT[:, l:l + 1],
                                                                         start=(l == 0), stop=(l == 31)))
            kb.op('act', [B1], [b1], lambda en: en.copy(out=b1[:], in_=B1[:, :]))
            for g in range(2):
                gs = slice(g * 64, (g + 1) * 64)
                for hc in range(2):
                    H_ = H1[hc]
                    for l in range(32):
                        kb.op('pe', [W1, cT], [H_], lambda en, l=l: en.matmul(H_[:, 0:255], lhsT=W1[gs, l, hc * 128:(hc + 1) * 128],
                                                                             rhs=cT[gs, l:l + 16 * 254 + 1:16], start=(l == 0), stop=(l == 31)))
                    xx_, x2_, th_ = xx[hc], x2[hc], th[hc]
                    kb.op('act', [H_, b1], [xx_], lambda en: en.activation(out=xx_[:], in_=H_[:, 0:255], func=AF.Identity, bias=b1[:, hc:hc + 1]))
                    kb.op('act', [xx_], [x2_], lambda en: en.activation(out=x2_[:], in_=xx_[:], func=AF.Square))
                    kb.op('dve', [x2_], [x2_], lambda en: en.tensor_scalar(out=x2_[:], in0=x2_[:], scalar1=0.044715, scalar2=1.0, op0=ALU.mult, op1=ALU.add))
                    kb.op('dve', [x2_, xx_], [x2_], lambda en: en.tensor_tensor(out=x2_[:], in0=x2_[:], in1=xx_[:], op=ALU.mult))
                    kb.op('act', [x2_], [th_], lambda en: en.activation(out=th_[:], in_=x2_[:], func=AF.Tanh, scale=0.7978845608028654))
                    kb.op('dve', [th_], [th_], lambda en: en.tensor_scalar(out=th_[:], in0=th_[:], scalar1=1.0, scalar2=0.5, op0=ALU.add, op1=ALU.mult))
                    kb.op('dve', [th_, xx_], [g1T], lambda en: en.tensor_tensor(out=g1T[:, hc, 0:255], in0=th_[:], in1=xx_[:], op=ALU.mult))
                for ct in range(2):
                    O_ = O2[ct]
                    for hc in range(2):
                        kb.op('pe', [g1T, W2], [O_], lambda en, hc=hc: en.matmul(O_[:, :], lhsT=g1T[:, hc, ct * 128:(ct + 1) * 128], rhs=W2[:, hc, :],
                                                                                start=(hc == 0), stop=(hc == 1)))
                    if kv == 1:
                        kb.op('act', [O_], [vc], lambda en: en.copy(out=vc[:, ct, g, :], in_=O_[:, :]))
                    else:
                        r = ct
                        kb.op('pool', [], [ssq[r]], lambda en: en.memset(ssq[r][:], 0.0))
                        kb.op('act', [O_], [junk, ssq[r]], lambda en: en.activation(out=junk[:], in_=O_[:, :], func=AF.Square, accum_out=ssq[r][:, 0:1]))
                        kb.op('act', [ssq[r]], [rstd[r]], lambda en: en.activation(out=rstd[r][:], in_=ssq[r][:], func=AF.Sqrt, bias=EPS, scale=1.0 / 64))
                        kb.op('dve', [rstd[r]], [rstd[r]], lambda en: en.reciprocal(out=rstd[r][:], in_=rstd[r][:]))
                        kb.op('dve', [O_, rstd[r]], [kn[r]], lambda en: en.tensor_scalar(out=kn[r][:], in0=O_[:, :], scalar1=rstd[r][:, 0:1], scalar2=None, op0=ALU.mult))
                        kb.op('pe', [kn[r], ident], [T0], lambda en: en.transpose(out=T0[0:64, ct, :], in_=kn[r][:], identity=ident[:]))
                        n = 128 if ct == 0 else 127
                        kb.op('act', [T0, kcol], [kcT], lambda en: en.activation(out=kcT[:, g, ct * 128:ct * 128 + n], in_=T0[0:64, ct, 0:n], func=AF.Copy,
                                                                                 scale=kcol[:, 0:1]))
        kb.dma('sp', d['nsa_kcT'][:, :, :], kcT[:], [kcT], [d['nsa_kcT_b']], kcT)
        kb.dma('sp', d['nsa_vc'][:, :, :, :], vc[:], [vc], [d['nsa_vc_b']], vc)


def phase_nsa(kb, d, qts=None, groups=(0, 1)):
    qts = list(range(NT)) if qts is None else qts
    with kb.phase() as ph:
        ident = make_ident(kb, ph, d)
        QT = ph.sb('QT', [128, 4, S], BF16)
        KsT = ph.sb('KsT', [128, S], BF16)
        KwT = ph.sb('KwT', [128, S], BF16)
        kcT = ph.sb('kcT', [128, 256], BF16)
        Vs = ph.sb('Vs', [128, NT, 65], BF16)
        Vw = ph.sb('Vw', [128, NT, 65], BF16)
        Vc = ph.sb('Vc', [128, 2, 129], BF16)
        ovs = ph.sb('ovs', [128, 2, 64], F32)
        exs = ph.sb('exs', [64, 1024], F32)
        Ex = ph.sb('Ex', [128, S], BF16)
        bts = ph.sb('bts', [128, 4, 4, 128], F32)
        btm = ph.sb('btm', [128, 4, 128], F32)
        Bt = ph.sb('Bt', [128, 4, 512], BF16)
        cbs = ring(ph, 'cbs', [128, 2, 4, 128], F32)
        cbm = ring(ph, 'cbm', [128, 2, 128], F32)
        CB = ring(ph, 'CB', [128, 2, 512], BF16)
        frc = ring(ph, 'frc', [128, 64], F32)
        gt = ring(ph, 'gt', [128, 24], F32)
        Pr = ring(ph, 'P', [128, 512], BF16, 3)
        osb = ring(ph, 'osb', [128, 4, 129], F32)
        den = ring(ph, 'den', [128, 4], F32)
        fac = ring(ph, 'fac', [128, 4], F32)
        oacc = ring(ph, 'oacc', [128, 4, 64], F32)
        imp = ring(ph, 'imp', [128, 64], F32)
        imp3 = ring(ph, 'imp3', [128, 64], F32)
        m8 = ring(ph, 'm8', [128, 16], F32)
        selb = ring(ph, 'selb', [128, 64], F32)
        nsb = ring(ph, 'nsb', [128, 64], BF16)
        NS = ring(ph, 'NS', [128, 4, 128], BF16)
        om = ring(ph, 'om', [128, 256], BF16)
        Sr = [ph.ps('S%d' % i, [128, 512], F32) for i in range(2)]
        Oa = [ph.ps('Oa%d' % i, [128, 129], F32) for i in range(4)]
        Tn = ph.ps('Tn', [128, 8, 128], BF16)
        for b in (QT, KsT, KwT, kcT, Ex, NS[0], NS[1]):
            kb.op('pool', [], [b], lambda en, b=b: en.memset(b[64:128], 0.0))
        kb.dma('sp', btm[:], d['c_btmask'][:, :, :], [], [btm], btm)
        kb.dma('sp', ovs[:], d['c_ov'][:, :, :], [], [ovs], ovs)
        for i in range(4):
            kb.dma('sp', exs[:], d['c_ex'][:, i * 1024:(i + 1) * 1024], [], [exs], exs)
            kb.op('dve', [exs], [Ex], lambda en, i=i: en.tensor_copy(out=Ex[0:64, i * 1024:(i + 1) * 1024], in_=exs[:]))
        kb.op('pool', [], [Vs], lambda en: en.memset(Vs[:], 1.0))
        kb.op('pool', [], [Vw], lambda en: en.memset(Vw[:], 1.0))
        kb.op('pool', [], [Vc], lambda en: en.memset(Vc[:], 1.0))
        kb.op('dve', [ovs], [Vc], lambda en: en.tensor_copy(out=Vc[:, :, 65:129], in_=ovs[:]))
        it = 0
        itq = 0
        for g in groups:
            kb.dma('sp', QT[0:64], d['nsa_qT'][:, g * 4:(g + 1) * 4, :], [d['nsa_qT_b']], [QT], QT)
            kb.dma('act', KsT[0:64], d['nsa_kT'][:, g, :], [d['nsa_kT_b']], [KsT], KsT)
            kb.dma('act', KwT[0:64], d['nsa_kT'][:, 2 + g, :], [d['nsa_kT_b']], [KwT], KwT)
            kb.dma('sp', kcT[0:64], d['nsa_kcT'][:, g, :], [d['nsa_kcT_b']], [kcT], kcT)
            kb.dma('sp', Vs[:, :, 0:64], d['nsa_v'][:, g, :].rearrange('(t p) e -> p t e', p=128), [d['nsa_v_b']], [Vs], Vs)
            kb.dma('act', Vw[:, :, 0:64], d['nsa_v'][:, 2 + g, :].rearrange('(t p) e -> p t e', p=128), [d['nsa_v_b']], [Vw], Vw)
            kb.dma('sp', Vc[:, :, 0:64], d['nsa_vc'][:, :, g, :], [d['nsa_vc_b']], [Vc], Vc)
            kb.dma('sp', bts[:], d['nsa_bt'][g], [], [bts], bts)
            kb.op('dve', [bts, btm], [Bt], lambda en: en.tensor_tensor(out=Bt[:].rearrange('p t (r q) -> p t r q', r=4), in0=bts[:],
                                                                      in1=btm[:].unsqueeze(2).broadcast_to([128, 4, 4, 128]), op=ALU.add))
            def prefetch(qt, r2):
                kb.dma('sp', cbs[r2][:], d['nsa_cbias'][qt, g], [], [cbs[r2]], cbs[r2])
                kb.dma('sp', cbm[r2][:], d['c_cmask'][qt], [], [cbm[r2]], cbm[r2])
                kb.dma('sp', frc[r2][:], d['c_force'][qt], [], [frc[r2]], frc[r2])
                kb.dma('sp', gt[r2][:], d['gates'][qt * 128:(qt + 1) * 128, :], [d['gates_b']], [gt[r2]], gt[r2])
                kb.op('dve', [cbs[r2], cbm[r2]], [CB[r2]], lambda en: en.tensor_tensor(
                    out=CB[r2][:].rearrange('p t (r q) -> p t r q', r=4), in0=cbs[r2][:], in1=cbm[r2][:].unsqueeze(2).broadcast_to([128, 2, 4, 128]), op=ALU.add))
            prefetch(qts[0], itq % 2)
            for qi, qt in enumerate(qts):
                r2 = itq % 2
                itq += 1
                qs = slice(qt * 128, (qt + 1) * 128)
                CB_ = CB[r2]
                rhsq = QT[:, :, qs]
                oacc_, osb_, den_, fac_, gt_ = oacc[r2], osb[r2], den[r2], fac[r2], gt[r2]

                def branch(tiles, width, bi):
                    nonlocal it
                    n = len(tiles)
                    slots = {}

                    def emit_s(ti):
                        nonlocal it
                        S_, P_ = Sr[it % 2], Pr[it % 3]
                        it += 1
                        slots[ti] = (S_, P_)
                        mms = tiles[ti][0]
                        for mi, (lh, rh, rd) in enumerate(mms):
                            kb.op('pe', rd, [S_], lambda en: en.matmul(S_[:, :], lhsT=lh, rhs=rh, start=(mi == 0), stop=(mi == len(mms) - 1)))
                    emit_s(0)
                    for ti, (mms, vrhs, vbuf) in enumerate(tiles):
                        if ti + 1 < n:
                            emit_s(ti + 1)
                        S_, P_ = slots[ti]
                        kb.op('act', [S_], [P_], lambda en: en.activation(out=P_[:], in_=S_[:, :], func=AF.Exp))
                        for r in range(4):
                            kb.op('pe', [P_, vbuf], [Oa[r]], lambda en, r=r: en.matmul(Oa[r][:, 0:width], lhsT=P_[:, r * 128:(r + 1) * 128], rhs=vrhs,
                                                                                      start=(ti == 0), stop=(ti == n - 1)))
                    for r in range(4):
                        kb.op('act', [Oa[r]], [osb_], lambda en, r=r: en.copy(out=osb_[:, r, 0:width], in_=Oa[r][:, 0:width]))
                    kb.op('dve', [osb_], [den_], lambda en: en.tensor_scalar(out=den_[:], in0=osb_[:, :, 64], scalar1=1e-30, scalar2=None, op0=ALU.max))
                    kb.op('dve', [den_], [den_], lambda en: en.reciprocal(out=den_[:], in_=den_[:]))
                    kb.op('dve', [den_, gt_], [fac_], lambda en: en.tensor_tensor(out=fac_[:], in0=den_[:], in1=gt_[:, g * 12 + bi:g * 12 + 12:3], op=ALU.mult))
                    if bi == 0:
                        kb.op('dve', [osb_, fac_], [oacc_], lambda en: en.tensor_tensor(out=oacc_[:], in0=osb_[:, :, 0:64], in1=bcast_mid(fac_[:], 64), op=ALU.mult))
                    else:
                        tmp = imp3[r2]
                        for r in range(4):
                            kb.op('dve', [osb_, fac_, oacc_], [oacc_], lambda en, r=r: en.scalar_tensor_tensor(
                                out=oacc_[:, r, :], in0=osb_[:, r, 0:64], scalar=fac_[:, r:r + 1], in1=oacc_[:, r, :], op0=ALU.mult, op1=ALU.add))

                nct = 1 if qt <= 15 else 2
                tiles = []
                for ct in range(nct):
                    tiles.append(([(kcT[:, ct * 128:(ct + 1) * 128], rhsq, [kcT, QT]), (ident[:], CB_[:, ct, :], [ident, CB_])], Vc[:, ct, :], Vc))
                branch(tiles, 129, 0)
                imp_, imp3_, m8_, selb_, nsb_, NS_ = imp[r2], imp3[r2], m8[r2], selb[r2], nsb[r2], NS[r2]
                kb.op('dve', [osb_, den_], [imp_], lambda en: en.tensor_scalar(out=imp_[:], in0=osb_[:, 0, 65:129], scalar1=den_[:, 0:1], scalar2=None, op0=ALU.mult))
                for r in range(1, 4):
                    kb.op('dve', [osb_, den_, imp_], [imp_], lambda en, r=r: en.scalar_tensor_tensor(
                        out=imp_[:], in0=osb_[:, r, 65:129], scalar=den_[:, r:r + 1], in1=imp_[:], op0=ALU.mult, op1=ALU.add))
                kb.op('dve', [imp_, frc[r2]], [imp_], lambda en: en.tensor_tensor(out=imp_[:], in0=imp_[:], in1=frc[r2][:], op=ALU.max))
                kb.op('dve', [imp_], [m8_], lambda en: en.max(out=m8_[:, 0:8], in_=imp_[:]))
                kb.op('dve', [imp_, m8_], [imp3_], lambda en: en.match_replace(out=imp3_[:], in_to_replace=m8_[:, 0:8], in_values=imp_[:], imm_value=-1.0))
                kb.op('dve', [imp3_], [m8_], lambda en: en.max(out=m8_[:, 8:16], in_=imp3_[:]))
                kb.op('dve', [imp_, m8_], [selb_], lambda en: en.tensor_scalar(out=selb_[:], in0=imp_[:], scalar1=m8_[:, 15:16], scalar2=None, op0=ALU.is_ge))
                kb.op('dve', [selb_], [nsb_], lambda en: en.tensor_scalar(out=nsb_[:], in0=selb_[:], scalar1=-1.0, scalar2=30000.0, op0=ALU.add, op1=ALU.mult))
                tiles = []
                for kt in range(max(0, qt - 4), qt + 1):
                    ty = 0 if kt == qt else (1 if kt == qt - 1 else (3 if kt == qt - 4 else 2))
                    ks = slice(kt * 128, (kt + 1) * 128)
                    tiles.append(([(KwT[:, ks], rhsq, [KwT, QT]), (ident[:], Bt[:, ty, :], [ident, Bt])], Vw[:, kt, :], Vw))
                branch(tiles, 65, 2)
                kb.op('pe', [nsb_, ident], [Tn], lambda en: en.transpose(out=Tn[0:64, 0, :], in_=nsb_[:], identity=ident[:]))
                for r in range(4):
                    kb.op('act', [Tn], [NS_], lambda en, r=r: en.copy(out=NS_[0:64, r, :], in_=Tn[0:64, 0, :]))
                if qi + 1 < len(qts):
                    prefetch(qts[qi + 1], itq % 2)
                tiles = []
                for kt in range(0, qt + 1):
                    ty = 0 if kt == qt else (1 if kt == qt - 1 else 2)
                    ks = slice(kt * 128, (kt + 1) * 128)
                    tiles.append(([(KsT[:, ks], rhsq, [KsT, QT]), (ident[:], Bt[:, ty, :], [ident, Bt]),
                                   (Ex[:, ks], NS_[:].rearrange('p r q -> p (r q)'), [Ex, NS_])], Vs[:, kt, :], Vs))
                branch(tiles, 65, 1)
                om_ = om[r2]
                kb.op('act', [oacc_], [om_], lambda en: en.copy(out=om_[:], in_=oacc_[:].rearrange('p r e -> p (r e)')))
                kb.dma('act', d['mix'][qs, g * 256:(g + 1) * 256], om_[:], [om_], [d['mix_b']], om_)


def build(phases=('e1',), debug_out=(), nt=NT, lim=99, ext_in=(), **kw):
    nc = bass.Bass("TRN2", target_bir_lowering=False)
    d = {}
    for name, shape in INPUTS.items():
        d[name] = nc.dram_tensor(name, shape, F32, kind="ExternalInput")
    for name, (shape, dt) in SCRATCH.items():
        kind = "ExternalOutput" if name in debug_out else ("ExternalInput" if name in ext_in else "Internal")
        d[name] = nc.dram_tensor(name, shape, dt, kind=kind)
        d[name + '_b'] = Buf(name, d[name], multi=True)
    d['out'] = nc.dram_tensor('out', [S, D], F32, kind="ExternalOutput")
    d['out_b'] = Buf('out', d['out'], multi=True)
    with ExitStack() as es:
        kb = KB(nc, es)
        if 'e1' in phases:
            phase_e1(kb, d, nt, lim)
        if 'mla' in phases:
            phase_mla(kb, d, kw.get('nqc', 8))
        if 'cmp' in phases:
            phase_cmp(kb, d)
        if 'nsa' in phases:
            phase_nsa(kb, d, kw.get('qts'), kw.get('groups', (0, 1)))
        if 'wout' in phases:
            phase_wout(kb, d, nt)
        if 'ffn' in phases:
            ps = []
            for i, (rsd, dst) in enumerate((('x1a', 'x1h'), ('x1h', 'x1'))):
                ps.append(dict(src='x1a', resid=rsd, dst=dst, grow=d['ev_ffn_row'], gate=None,
                               wg=d['ffn_w_gate'][:, i * DFE:(i + 1) * DFE], wu=d['ffn_w_up'][:, i * DFE:(i + 1) * DFE],
                               wd=d['ffn_w_down'][i * DFE:(i + 1) * DFE, :]))
            expert_passes(kb, d, ps, kw.get('nst', 8))
        if 'odd' in phases:
            phase_odd_mixer(kb, d, kw.get('nst', 8))
        if 'router' in phases:
            phase_router(kb, d, nt)
        if 'moe' in phases:
            ps = []
            ne = kw.get('ne', NE)
            chain = ['x2a'] + [('acc0', 'acc1')[i % 2] for i in range(ne - 1)] + ['out']
            for e in range(ne):
                ps.append(dict(src='x2a', resid=chain[e], dst=chain[e + 1], grow=d['od_ffn_row'], gate=('moe_gate', e),
                               wg=d['moe_w_gate'][e], wu=d['moe_w_up'][e], wd=d['moe_w_down'][e]))
            expert_passes(kb, d, ps, kw.get('nst', 8))
    return nc


ALL_PHASES = ('e1', 'cmp', 'nsa', 'mla', 'wout', 'ffn', 'odd', 'moe')
_NC_CACHE = {}


def kernel(**inputs):
    inp = {k: np.asarray(v) for k, v in inputs.items()}
    if 'nc' not in _NC_CACHE:
        _NC_CACHE['nc'] = build(ALL_PHASES)
    nc = _NC_CACHE['nc']
    consts = host_consts()
    n = inp['x'].shape[0]
    in_maps = [prep_core_inputs(inp, b, consts) for b in range(n)]
    res = run_bass_kernel_spmd(nc, in_maps, core_ids=list(range(n)))
    return np.stack([np.asarray(r['out'], dtype=np.float32) for r in res.results], axis=0)
```

```python
import math
from contextlib import ExitStack, contextmanager
import numpy as np
import concourse.bass as bass
import concourse.mybir as mybir
from concourse.bass_utils import run_bass_kernel_spmd

F32, BF16 = mybir.dt.float32, mybir.dt.bfloat16
AF = mybir.ActivationFunctionType
ALU = mybir.AluOpType
AX = mybir.AxisListType

S = 4096
D = 1024
NT = S // 128
EPS = 1e-6
EVEN_W = 1720
D_FF = 2816
NE = 8
DFE = 1408


class Buf:
    def __init__(self, name, h, multi=False):
        self.name = name
        self.h = h
        self.w = None
        self.wm = {} if multi else None
        self.r = {}
        self.lane = None

    def __getitem__(self, key):
        return self.h[key]


class KB:
    def __init__(self, nc, es, n_lanes=72):
        self.nc = nc
        self.eng = {'pe': nc.tensor, 'act': nc.scalar, 'dve': nc.vector, 'pool': nc.gpsimd, 'sp': nc.sync}
        self.sems = {}
        self.cnt = {}
        for e in ('pe', 'act', 'dve', 'pool'):
            self.sems[e] = es.enter_context(nc.semaphore('sem_' + e))
            self.cnt[e] = 0
        self.free_lanes = []
        self.free_sw_lanes = []
        for i in range(n_lanes):
            key = 'lane%d' % i
            self.sems[key] = es.enter_context(nc.semaphore(key))
            self.cnt[key] = 0
            (self.free_sw_lanes if i < 16 else self.free_lanes).append(key)
        self.seen = {e: {} for e in self.eng}
        self.phase_bufs = []
        self.uid = 0

    def _need(self, e, stamp):
        if stamp is None:
            return
        sk, v, _ = stamp
        if self.seen[e].get(sk, 0) >= v:
            return
        self.eng[e].wait_ge(self.sems[sk], v)
        self.seen[e][sk] = v

    def _deps(self, e, reads, writes, is_dma=False):
        for b in reads:
            self._need(e, b.w)
            if b.wm:
                for st in b.wm.values():
                    self._need(e, st)
        for b in writes:
            if b.w is not None and (is_dma or b.w[2] != e or e != 'pe'):
                self._need(e, b.w)
            for st in b.r.values():
                if is_dma or st[2] != e or e != 'pe':
                    self._need(e, st)

    def op(self, e, reads, writes, fn):
        self._deps(e, reads, writes)
        ins = fn(self.eng[e])
        self.cnt[e] += 1
        ins.then_inc(self.sems[e], 1)
        st = (e, self.cnt[e], e)
        for b in reads:
            b.r[e] = st
        for b in writes:
            b.w = st
            b.r = {}
        return ins

    def dma(self, q, out, in_, reads, writes, owner, **kw):
        self._deps(q, reads, writes, is_dma=True)
        sw = q == 'pool'
        attr = 'lane_sw' if sw else 'lane'
        if getattr(owner, attr, None) is None:
            lane = (self.free_sw_lanes if sw else self.free_lanes).pop()
            setattr(owner, attr, lane)
            (self.phase_sw_lanes if sw else self.phase_lanes).append(lane)
        lk = getattr(owner, attr)
        self.eng[q].dma_start(out=out, in_=in_, **kw).then_inc(self.sems[lk], 16)
        self.cnt[lk] += 16
        st = (lk, self.cnt[lk], None)
        for b in reads:
            b.r[lk] = st
        for b in writes:
            if b.wm is not None:
                b.wm[lk] = st
            else:
                b.w = st
                b.r = {}

    def barrier(self):
        keys = [k for k in self.sems if self.cnt[k] > 0]
        for e in self.eng:
            for k in keys:
                if k == e:
                    continue
                self._need(e, (k, self.cnt[k], None))

    @contextmanager
    def phase(self):
        self.phase_lanes = []
        self.phase_sw_lanes = []
        with ExitStack() as es:
            ph = Phase(self, es)
            yield ph
            self.barrier()
        self.free_lanes.extend(self.phase_lanes)
        self.free_sw_lanes.extend(self.phase_sw_lanes)
        self.phase_lanes = []
        self.phase_sw_lanes = []


class Phase:
    def __init__(self, kb, es):
        self.kb = kb
        self.es = es

    def sb(self, name, shape, dt):
        self.kb.uid += 1
        h = self.es.enter_context(self.kb.nc.sbuf_tensor('%s_%d' % (name, self.kb.uid), shape, dt))
        return Buf(name, h)

    def ps(self, name, shape, dt):
        self.kb.uid += 1
        h = self.es.enter_context(self.kb.nc.psum_tensor('%s_%d' % (name, self.kb.uid), shape, dt))
        return Buf(name, h)


def bcast_mid(ap2d_rows, n):
    return ap2d_rows.unsqueeze(2).broadcast_to([ap2d_rows.shape[0], ap2d_rows.shape[1], n])


class Ctx:
    pass


def load_w(kb, ph, dst, src, K, N, stage=None, rowscale=None, **kw):
    assert rowscale is None
    for kc in range(K // 128):
        kb.dma('pool', dst[:, kc, :], src[kc * 128:(kc + 1) * 128, :], [], [dst], dst)


def rstd_from_ssq(kb, ssq, rstd, n, width):
    kb.op('act', [ssq], [rstd], lambda en: en.activation(out=rstd[:, 0:width], in_=ssq[:, 0:width], func=AF.Sqrt,
                                                         bias=EPS, scale=1.0 / n))
    kb.op('dve', [rstd], [rstd], lambda en: en.vector.reciprocal(out=rstd[:, 0:width], in_=rstd[:, 0:width])
          if False else en.reciprocal(out=rstd[:, 0:width], in_=rstd[:, 0:width]))


def ring(ph, name, shape, dt, n=2):
    return [ph.sb('%s%d' % (name, i), shape, dt) for i in range(n)]


def bc_row(t, n):
    return bass.AP(t, 0, [[0, 128], [1, n]])


def phase_e1(kb, d, nt=NT, lim=99):
    with kb.phase() as ph:
        identf = ph.sb('identf', [128, 128], F32)
        ident = ph.sb('ident', [128, 128], BF16)
        kb.dma('sp', identf[:], d['c_ident'][:, :], [], [identf], identf)
        kb.op('dve', [identf], [ident], lambda en: en.tensor_copy(out=ident[:], in_=identf[:]))
        Win = ph.sb('Win', [128, 8, EVEN_W], BF16)
        Wuq = ph.sb('Wuq', [128, 2, 768], BF16)
        Wukv = ph.sb('Wukv', [128, 1, 1024], BF16)
        qcol = ph.sb('qcol', [64, 1], F32)
        kcol = ph.sb('kcol', [64, 3], F32)
        mqg = ph.sb('mqg', [128, 768], F32)
        mkg = ph.sb('mkg', [128, 96], F32)
        for t, src in ((qcol, d['nsa_q_col']), (kcol, d['nsa_k_col'])):
            kb.dma('sp', t[:], src[:, :], [], [t], t)
        grow = ph.sb('grow', [128, D], F32)
        crow = ph.sb('crow', [128, 384], F32)
        kb.dma('sp', grow[:], bc_row(d['ev_mix_row'], D), [], [grow], grow)
        kb.dma('sp', crow[:], bc_row(d['cqkv_row'], 384), [], [crow], crow)
        kb.dma('sp', mqg[:], bc_row(d['mla_q_row'], 768), [], [mqg], mqg)
        kb.dma('sp', mkg[:], bc_row(d['mla_k_row'], 96), [], [mkg], mkg)
        kb.op('dve', [qcol], [qcol], lambda en: en.tensor_scalar(out=qcol[:], in0=qcol[:], scalar1=64 ** -0.5,
                                                                  scalar2=None, op0=ALU.mult))
        load_w(kb, ph, Win, d['ev_w_in'], 1024, EVEN_W)
        load_w(kb, ph, Wuq, d['mla_w_uq'], 256, 768)
        load_w(kb, ph, Wukv, d['mla_w_ukv'], 128, 1024)

        T0 = ph.ps('T0', [128, 8, 128], BF16)
        T1 = ph.ps('T1', [128, 8, 128], BF16)
        M = [ph.ps('M%d' % i, [128, 512], F32) for i in range(6)]
        xr = ring(ph, 'xr', [128, D], F32)
        junk = ph.sb('junk', [128, D], BF16)
        ssq = ring(ph, 'ssq', [128, 16], F32)
        rstd = ring(ph, 'rstd', [128, 16], F32)
        xs = ring(ph, 'xs', [128, D], BF16)
        xnT = ring(ph, 'xnT', [128, 8, 128], BF16)
        sq = ring(ph, 'sq', [128, 768], F32)
        qn = ring(ph, 'qn', [128, 8, 64], BF16)
        kn = ring(ph, 'kn', [128, 4, 64], BF16)
        cb = ring(ph, 'cb', [128, 256], BF16)
        vb = ring(ph, 'vb', [128, 4, 64], BF16)
        gt = ring(ph, 'gt', [128, 24], F32)
        cqn = ring(ph, 'cqn', [128, 384], BF16)
        krp = ring(ph, 'krp', [128, 32], F32)
        cT = ring(ph, 'cT', [128, 3, 128], BF16)
        qf = ring(ph, 'qf', [128, 8, 96], F32)
        rt = ring(ph, 'rt', [128, 4, 8, 16], F32)
        qb = ring(ph, 'qb', [128, 8, 96], BF16)
        kbf = ring(ph, 'kbf', [128, 8, 96], BF16)
        kt = ring(ph, 'kt', [128, 8, 64], F32)
        kr2 = ring(ph, 'kr2', [128, 2, 32], F32)
        vb2 = ring(ph, 'vb2', [128, 8, 64], BF16)
        cs = ring(ph, 'cs', [128, 32], F32)
        QT = ring(ph, 'QT', [64, 8, 512], BF16)
        KST = ring(ph, 'KST', [64, 4, 512], BF16)
        CT = ring(ph, 'CT', [128, 2, 512], BF16)
        MQT = ring(ph, 'MQT', [96, 8, 512], BF16)
        MKT = ring(ph, 'MKT', [96, 8, 512], BF16)
        chunks = ((0, 512), (512, 1024), (1024, 1304), (1304, 1720))

        for tt in range(nt):
            st, j = divmod(tt, 4)
            r = tt % 2
            s2 = st % 2
            cols = slice(j * 128, (j + 1) * 128)
            rows = slice(tt * 128, (tt + 1) * 128)
            x_, ssq_, rstd_, xs_, xnT_, sq_ = xr[r], ssq[r], rstd[r], xs[r], xnT[r], sq[r]
            kb.dma('sp', x_[:], d['x'][rows, :], [], [x_], x_)
            kb.dma('act', cs[r][:], d['c_cossin'][rows, :], [], [cs[r]], cs[r])
            if lim < 1:
                continue
            kb.op('pool', [], [ssq_], lambda en: en.memset(ssq_[:], 0.0))
            kb.op('act', [x_], [junk, ssq_], lambda en: en.activation(out=junk[:], in_=x_[:], func=AF.Square,
                                                                      accum_out=ssq_[:, 0:1]))
            kb.op('act', [ssq_], [rstd_], lambda en: en.activation(out=rstd_[:, 0:1], in_=ssq_[:, 0:1], func=AF.Sqrt,
                                                                   bias=EPS, scale=1.0 / D))
            kb.op('dve', [rstd_], [rstd_], lambda en: en.reciprocal(out=rstd_[:, 0:1], in_=rstd_[:, 0:1]))
            kb.op('dve', [x_, rstd_, grow], [xs_], lambda en: en.scalar_tensor_tensor(out=xs_[:], in0=x_[:], scalar=rstd_[:, 0:1], in1=grow[:],
                                                                                     op0=ALU.mult, op1=ALU.mult))
            if lim < 2:
                continue
            for k in range(8):
                kb.op('pe', [xs_, ident], [T0], lambda en, k=k: en.transpose(out=T0[:, k, :], in_=xs_[:, k * 128:(k + 1) * 128],
                                                                             identity=ident[:]))
            kb.op('act', [T0], [xnT_], lambda en: en.copy(out=xnT_[:], in_=T0[:]))
            if lim < 3:
                continue
            for ci, (c0, c1) in enumerate(chunks):
                for k in range(8):
                    kb.op('pe', [xnT_, Win], [M[ci]], lambda en, k=k, ci=ci, c0=c0, c1=c1: en.matmul(
                        M[ci][:, 0:c1 - c0], lhsT=xnT_[:, k, :], rhs=Win[:, k, c0:c1], start=(k == 0), stop=(k == 7)))
            hA, hB, hC, hD = M[0], M[1], M[2], M[3]
            if lim < 4:
                continue
            kb.op('act', [hA], [sq_], lambda en: en.activation(out=sq_[:, 0:512], in_=hA[:, 0:512], func=AF.Square))
            kb.op('dve', [sq_], [ssq_], lambda en: en.tensor_reduce(out=ssq_[:, 1:9], in_=sq_[:, 0:512].rearrange(
                'p (h d) -> p h d', h=8), axis=AX.X, op=ALU.add))
            kb.op('act', [ssq_], [rstd_], lambda en: en.activation(out=rstd_[:, 1:9], in_=ssq_[:, 1:9], func=AF.Sqrt,
                                                                   bias=EPS, scale=1.0 / 64))
            kb.op('dve', [rstd_], [rstd_], lambda en: en.reciprocal(out=rstd_[:, 1:9], in_=rstd_[:, 1:9]))
            qn_ = qn[r]
            kb.op('dve', [hA, rstd_], [qn_], lambda en: en.tensor_tensor(
                out=qn_[:], in0=hA[:, 0:512].rearrange('p (h d) -> p h d', h=8), in1=bcast_mid(rstd_[:, 1:9], 64), op=ALU.mult))
            for h in range(8):
                kb.op('pe', [qn_, ident], [T1], lambda en, h=h: en.transpose(out=T1[0:64, h, :], in_=qn_[:, h, :], identity=ident[:]))
            QT_ = QT[s2]
            kb.op('act', [T1, qcol], [QT_], lambda en: en.activation(out=QT_[:, :, cols], in_=T1[0:64, :, :], func=AF.Copy,
                                                                     scale=qcol[:, 0:1]))
            if lim < 5:
                continue
            kb.op('act', [hB], [sq_], lambda en: en.activation(out=sq_[:, 0:128], in_=hB[:, 256:384], func=AF.Square))
            kb.op('act', [hC], [sq_], lambda en: en.activation(out=sq_[:, 128:256], in_=hC[:, 0:128], func=AF.Square))
            kb.op('dve', [sq_], [ssq_], lambda en: en.tensor_reduce(out=ssq_[:, 9:13], in_=sq_[:, 0:256].rearrange(
                'p (h d) -> p h d', h=4), axis=AX.X, op=ALU.add))
            kb.op('act', [ssq_], [rstd_], lambda en: en.activation(out=rstd_[:, 9:13], in_=ssq_[:, 9:13], func=AF.Sqrt,
                                                                   bias=EPS, scale=1.0 / 64))
            kb.op('dve', [rstd_], [rstd_], lambda en: en.reciprocal(out=rstd_[:, 9:13], in_=rstd_[:, 9:13]))
            kn_ = kn[r]
            kb.op('dve', [hB, rstd_], [kn_], lambda en: en.tensor_tensor(
                out=kn_[:, 0:2, :], in0=hB[:, 256:384].rearrange('p (h d) -> p h d', h=2), in1=bcast_mid(rstd_[:, 9:11], 64), op=ALU.mult))
            kb.op('dve', [hC, rstd_], [kn_], lambda en: en.tensor_tensor(
                out=kn_[:, 2:4, :], in0=hC[:, 0:128].rearrange('p (h d) -> p h d', h=2), in1=bcast_mid(rstd_[:, 11:13], 64), op=ALU.mult))
            cb_ = cb[r]
            kb.op('dve', [hB], [cb_], lambda en: en.tensor_copy(out=cb_[:], in_=hB[:, 0:256]))
            vb_ = vb[r]
            kb.op('act', [hB], [vb_], lambda en: en.copy(out=vb_[:, 0:2, :], in_=hB[:, 384:512].rearrange('p (h d) -> p h d', h=2)))
            kb.op('act', [hC], [vb_], lambda en: en.copy(out=vb_[:, 2:4, :], in_=hC[:, 128:256].rearrange('p (h d) -> p h d', h=2)))
            kb.dma('sp', d['nsa_v'][rows, :, :], vb_[:], [vb_], [d['nsa_v_b']], vb_)
            gt_ = gt[r]
            kb.op('act', [hC], [gt_], lambda en: en.activation(out=gt_[:], in_=hC[:, 256:280], func=AF.Sigmoid))
            kb.dma('sp', d['gates'][rows, :], gt_[:], [gt_], [d['gates_b']], gt_)
            for i in range(4):
                kb.op('pe', [kn_, ident], [T0], lambda en, i=i: en.transpose(out=T0[0:64, i, :], in_=kn_[:, i, :], identity=ident[:]))
            for i in range(2):
                kb.op('pe', [cb_, ident], [T0], lambda en, i=i: en.transpose(out=T0[:, 4 + i, :], in_=cb_[:, i * 128:(i + 1) * 128],
                                                                             identity=ident[:]))
            KST_ = KST[s2]
            kb.op('act', [T0, kcol], [KST_], lambda en: en.activation(out=KST_[:, 0:2, cols], in_=T0[0:64, 0:2, :], func=AF.Copy,
                                                                      scale=kcol[:, 1:2]))
            kb.op('act', [T0, kcol], [KST_], lambda en: en.activation(out=KST_[:, 2:4, cols], in_=T0[0:64, 2:4, :], func=AF.Copy,
                                                                      scale=kcol[:, 2:3]))
            CT_ = CT[s2]
            kb.op('act', [T0], [CT_], lambda en: en.copy(out=CT_[:, :, cols], in_=T0[:, 4:6, :]))
            if lim < 6:
                continue
            kb.op('pool', [], [ssq_], lambda en: en.memset(ssq_[:, 13:16], 0.0))
            kb.op('act', [hD], [sq_, ssq_], lambda en: en.activation(out=sq_[:, 0:256], in_=hD[:, 0:256], func=AF.Square,
                                                                     accum_out=ssq_[:, 13:14]))
            kb.op('act', [hD], [sq_, ssq_], lambda en: en.activation(out=sq_[:, 256:384], in_=hD[:, 256:384], func=AF.Square,
                                                                     accum_out=ssq_[:, 14:15]))
            kb.op('act', [hD], [sq_, ssq_], lambda en: en.activation(out=sq_[:, 384:416], in_=hD[:, 384:416], func=AF.Square,
                                                                     accum_out=ssq_[:, 15:16]))
            kb.op('act', [ssq_], [rstd_], lambda en: en.activation(out=rstd_[:, 13:14], in_=ssq_[:, 13:14], func=AF.Sqrt,
                                                                   bias=EPS, scale=1.0 / 256))
            kb.op('act', [ssq_], [rstd_], lambda en: en.activation(out=rstd_[:, 14:15], in_=ssq_[:, 14:15], func=AF.Sqrt,
                                                                   bias=EPS, scale=1.0 / 128))
            kb.op('dve', [rstd_], [rstd_], lambda en: en.reciprocal(out=rstd_[:, 13:15], in_=rstd_[:, 13:15]))
            cqn_ = cqn[r]
            kb.op('dve', [hD, rstd_, crow], [cqn_], lambda en: en.scalar_tensor_tensor(out=cqn_[:, 0:256], in0=hD[:, 0:256], scalar=rstd_[:, 13:14],
                                                                                      in1=crow[:, 0:256], op0=ALU.mult, op1=ALU.mult))
            kb.op('dve', [hD, rstd_, crow], [cqn_], lambda en: en.scalar_tensor_tensor(out=cqn_[:, 256:384], in0=hD[:, 256:384], scalar=rstd_[:, 14:15],
                                                                                      in1=crow[:, 256:384], op0=ALU.mult, op1=ALU.mult))
            krp_ = krp[r]
            kb.op('dve', [hD, mkg], [krp_], lambda en: en.tensor_tensor(out=krp_[:], in0=hD[:, 384:416], in1=mkg[:, 64:96], op=ALU.mult))
            for i in range(3):
                kb.op('pe', [cqn_, ident], [T1], lambda en, i=i: en.transpose(out=T1[:, i, :], in_=cqn_[:, i * 128:(i + 1) * 128],
                                                                              identity=ident[:]))
            cT_ = cT[r]
            kb.op('act', [T1], [cT_], lambda en: en.copy(out=cT_[:], in_=T1[:, 0:3, :]))
            for c in range(2):
                for k in range(2):
                    kb.op('pe', [cT_, Wuq], [M[4 + c]], lambda en, c=c, k=k: en.matmul(
                        M[4 + c][:, 0:384], lhsT=cT_[:, k, :], rhs=Wuq[:, k, c * 384:(c + 1) * 384], start=(k == 0), stop=(k == 1)))
            for c in range(2):
                kb.op('act', [M[4 + c]], [sq_], lambda en, c=c: en.activation(out=sq_[:, c * 384:(c + 1) * 384], in_=M[4 + c][:, 0:384],
                                                                              func=AF.Square))
            kb.op('dve', [sq_], [ssq_], lambda en: en.tensor_reduce(out=ssq_[:, 1:9], in_=sq_[:, 0:768].rearrange(
                'p (h d) -> p h d', h=8), axis=AX.X, op=ALU.add))
            kb.op('act', [ssq_], [rstd_], lambda en: en.activation(out=rstd_[:, 1:9], in_=ssq_[:, 1:9], func=AF.Sqrt,
                                                                   bias=EPS, scale=1.0 / 96))
            kb.op('dve', [rstd_], [rstd_], lambda en: en.reciprocal(out=rstd_[:, 1:9], in_=rstd_[:, 1:9]))
            qf_ = qf[r]
            for c in range(2):
                kb.op('dve', [M[4 + c], rstd_], [qf_], lambda en, c=c: en.tensor_tensor(
                    out=qf_[:, c * 4:(c + 1) * 4, :], in0=M[4 + c][:, 0:384].rearrange('p (h d) -> p h d', h=4),
                    in1=bcast_mid(rstd_[:, 1 + c * 4:5 + c * 4], 96), op=ALU.mult))
            kb.op('pool', [qf_, mqg], [qf_], lambda en: en.tensor_tensor(out=qf_[:], in0=qf_[:], in1=mqg[:].rearrange(
                'p (h d) -> p h d', h=8), op=ALU.mult))
            cs_ = cs[r]
            rt_ = rt[r]
            qb_ = qb[r]

            def rope_bc(a, nh):
                return a.unsqueeze(1).broadcast_to([128, nh, 16])
            cosb, sinb = rope_bc(cs_[:, 0:16], 8), rope_bc(cs_[:, 16:32], 8)
            x1, x2 = qf_[:, :, 64:80], qf_[:, :, 80:96]
            kb.op('pool', [qf_, cs_], [rt_], lambda en: en.tensor_tensor(out=rt_[:, 0], in0=x1, in1=cosb, op=ALU.mult))
            kb.op('pool', [qf_, cs_], [rt_], lambda en: en.tensor_tensor(out=rt_[:, 1], in0=x2, in1=sinb, op=ALU.mult))
            kb.op('dve', [qf_, cs_], [rt_], lambda en: en.tensor_tensor(out=rt_[:, 2], in0=x1, in1=sinb, op=ALU.mult))
            kb.op('dve', [qf_, cs_], [rt_], lambda en: en.tensor_tensor(out=rt_[:, 3], in0=x2, in1=cosb, op=ALU.mult))
            kb.op('dve', [rt_], [qb_], lambda en: en.tensor_tensor(out=qb_[:, :, 64:80], in0=rt_[:, 0], in1=rt_[:, 1], op=ALU.subtract))
            kb.op('dve', [rt_], [qb_], lambda en: en.tensor_tensor(out=qb_[:, :, 80:96], in0=rt_[:, 2], in1=rt_[:, 3], op=ALU.add))
            kb.op('pool', [qf_], [qb_], lambda en: en.tensor_copy(out=qb_[:, :, 0:64], in_=qf_[:, :, 0:64]))
            for h in range(8):
                kb.op('pe', [qb_, ident], [T1], lambda en, h=h: en.transpose(out=T1[0:96, h, :], in_=qb_[:, h, :], identity=ident[:]))
            MQT_ = MQT[s2]
            kb.op('act', [T1], [MQT_], lambda en: en.activation(out=MQT_[:, :, cols], in_=T1[0:96, :, :], func=AF.Copy,
                                                                scale=96 ** -0.5))
            if lim < 7:
                continue
            for c in range(2):
                kb.op('pe', [cT_, Wukv], [M[c]], lambda en, c=c: en.matmul(
                    M[c][:, 0:512], lhsT=cT_[:, 2, :], rhs=Wukv[:, 0, c * 512:(c + 1) * 512], start=True, stop=True))
            for c in range(2):
                kb.op('act', [M[c]], [sq_], lambda en, c=c: en.activation(
                    out=sq_[:, c * 256:(c + 1) * 256].rearrange('p (h d) -> p h d', h=4),
                    in_=M[c][:, 0:512].rearrange('p (h d) -> p h d', h=4)[:, :, 0:64], func=AF.Square))
            kb.op('dve', [sq_], [ssq_], lambda en: en.tensor_reduce(out=ssq_[:, 1:9], in_=sq_[:, 0:512].rearrange(
                'p (h d) -> p h d', h=8), axis=AX.X, op=ALU.add))
            kb.op('dve', [ssq_], [ssq_], lambda en: en.tensor_scalar(out=ssq_[:, 1:9], in0=ssq_[:, 1:9], scalar1=ssq_[:, 15:16],
                                                                     scalar2=None, op0=ALU.add))
            kb.op('act', [ssq_], [rstd_], lambda en: en.activation(out=rstd_[:, 1:9], in_=ssq_[:, 1:9], func=AF.Sqrt,
                                                                   bias=EPS, scale=1.0 / 96))
            kb.op('dve', [rstd_], [rstd_], lambda en: en.reciprocal(out=rstd_[:, 1:9], in_=rstd_[:, 1:9]))
            kt_ = kt[r]
            kbf_ = kbf[r]
            vb2_ = vb2[r]
            for c in range(2):
                kb.op('dve', [M[c], rstd_], [kt_], lambda en, c=c: en.tensor_tensor(
                    out=kt_[:, c * 4:(c + 1) * 4, :], in0=M[c][:, 0:512].rearrange('p (h d) -> p h d', h=4)[:, :, 0:64],
                    in1=bcast_mid(rstd_[:, 1 + c * 4:5 + c * 4], 64), op=ALU.mult))
                kb.op('act', [M[c]], [vb2_], lambda en, c=c: en.copy(
                    out=vb2_[:, c * 4:(c + 1) * 4, :], in_=M[c][:, 0:512].rearrange('p (h d) -> p h d', h=4)[:, :, 64:128]))
            kb.dma('sp', d['mla_v'][rows, :, :], vb2_[:], [vb2_], [d['mla_v_b']], vb2_)
            kb.op('pool', [kt_, mkg], [kbf_], lambda en: en.tensor_tensor(
                out=kbf_[:, :, 0:64], in0=kt_[:], in1=mkg[:, 0:64].unsqueeze(1).broadcast_to([128, 8, 64]), op=ALU.mult))
            kr2_ = kr2[r]
            kb.op('pool', [krp_, cs_], [kr2_], lambda en: en.tensor_tensor(out=kr2_[:, 0, 0:16], in0=krp_[:, 0:16], in1=cs_[:, 0:16], op=ALU.mult))
            kb.op('pool', [krp_, cs_], [kr2_], lambda en: en.tensor_tensor(out=kr2_[:, 0, 16:32], in0=krp_[:, 0:16], in1=cs_[:, 16:32], op=ALU.mult))
            kb.op('pool', [krp_, cs_], [kr2_], lambda en: en.tensor_tensor(out=kr2_[:, 1, 0:16], in0=krp_[:, 16:32], in1=cs_[:, 16:32], op=ALU.mult))
            kb.op('pool', [krp_, cs_], [kr2_], lambda en: en.tensor_tensor(out=kr2_[:, 1, 16:32], in0=krp_[:, 16:32], in1=cs_[:, 0:16], op=ALU.mult))
            kb.op('pool', [kr2_], [kr2_], lambda en: en.tensor_tensor(out=kr2_[:, 0, 0:16], in0=kr2_[:, 0, 0:16], in1=kr2_[:, 1, 0:16], op=ALU.subtract))
            kb.op('pool', [kr2_], [kr2_], lambda en: en.tensor_tensor(out=kr2_[:, 0, 16:32], in0=kr2_[:, 0, 16:32], in1=kr2_[:, 1, 16:32], op=ALU.add))
            kb.op('dve', [kr2_, rstd_], [kbf_], lambda en: en.tensor_tensor(
                out=kbf_[:, :, 64:96], in0=kr2_[:, 0, :].unsqueeze(1).broadcast_to([128, 8, 32]), in1=bcast_mid(rstd_[:, 1:9], 32), op=ALU.mult))
            for h in range(8):
                kb.op('pe', [kbf_, ident], [T0], lambda en, h=h: en.transpose(out=T0[0:96, h, :], in_=kbf_[:, h, :], identity=ident[:]))
            MKT_ = MKT[s2]
            kb.op('act', [T0], [MKT_], lambda en: en.copy(out=MKT_[:, :, cols], in_=T0[0:96, :, :]))
            if lim < 8:
                continue
            if j == 3:
                sc = slice(st * 512, (st + 1) * 512)
                kb.dma('sp', d['nsa_qT'][:, :, sc], QT_[:], [QT_], [d['nsa_qT_b']], QT_)
                kb.dma('sp', d['nsa_kT'][:, :, sc], KST_[:], [KST_], [d['nsa_kT_b']], KST_)
                kb.dma('sp', d['nsa_cT'][:, :, sc], CT_[:], [CT_], [d['nsa_cT_b']], CT_)
                kb.dma('sp', d['mla_qT'][:, :, sc], MQT_[:], [MQT_], [d['mla_qT_b']], MQT_)
                kb.dma('sp', d['mla_kT'][:, :, sc], MKT_[:], [MKT_], [d['mla_kT_b']], MKT_)


SCRATCH = {
    'nsa_qT': ([64, 8, S], BF16), 'nsa_kT': ([64, 4, S], BF16), 'nsa_cT': ([128, 2, S], BF16),
    'nsa_v': ([S, 4, 64], BF16), 'gates': ([S, 24], F32),
    'mla_qT': ([96, 8, S], BF16), 'mla_kT': ([96, 8, S], BF16), 'mla_v': ([S, 8, 64], BF16),
    'mix': ([S, D], BF16), 'x1a': ([S, D], F32), 'x1h': ([S, D], F32), 'x1': ([S, D], F32),
    'x2a': ([S, D], F32), 'moe_gate': ([S, 8], F32), 'acc0': ([S, D], F32), 'acc1': ([S, D], F32),
    'nsa_kcT': ([64, 2, 256], BF16), 'nsa_vc': ([128, 2, 2, 64], BF16),
}
INPUTS = {
    'x': [S, D], 'c_ident': [128, 128], 'c_cossin': [S, 32],
    'ev_mix_row': [1, D], 'ev_w_in': [D, EVEN_W], 'nsa_q_col': [64, 1], 'nsa_k_col': [64, 3],
    'cqkv_row': [1, 384], 'mla_w_uq': [256, 768], 'mla_w_ukv': [128, 1024],
    'mla_q_row': [1, 768], 'mla_k_row': [1, 96], 'c_mla_mask': [128, 4, 512],
    'ev_w_out': [D, D], 'ev_ffn_row': [1, D], 'ffn_w_gate': [D, D_FF], 'ffn_w_up': [D, D_FF], 'ffn_w_down': [D_FF, D],
    'od_mix_row': [1, D], 'od_w_in': [D, 3 * D], 'conv_col': [128, 8, 3], 'od_w_out': [D, D],
    'od_ffn_row': [1, D], 'moe_wrT': [1, 8 * D], 'moe_b_row': [1, 8],
    'nsa_cmp_w1': [2, 2048, 256], 'nsa_cmp_w2': [2, 256, 64], 'nsa_posT': [2, 64, 32],
    'c_btmask': [128, 4, 128], 'c_ov': [128, 2, 64], 'c_ex': [64, S], 'nsa_bt': [2, 128, 4, 4, 128],
    'nsa_cbias': [NT, 2, 128, 2, 4, 128], 'c_cmask': [NT, 128, 2, 128], 'c_force': [NT, 128, 64],
    'moe_w_gate': [NE, D, DFE], 'moe_w_up': [NE, D, DFE], 'moe_w_down': [NE, DFE, D],
}


def col_layout(v, kc):
    return np.ascontiguousarray(np.asarray(v, np.float32).reshape(kc, 128).T)


def t5_bucket_np(dist):
    n = np.maximum(dist, 0)
    nf = np.maximum(n, 16).astype(np.float32)
    large = 16 + (np.log(nf / np.float32(16)) / np.float32(math.log(8.0)) * np.float32(16)).astype(np.int32)
    return np.where(n < 16, n, np.minimum(large, 31))


def nsa_index_tables():
    k = np.arange(128)[:, None]
    q = np.arange(128)[None, :]
    dists = [q - k, 128 + q - k, np.full((128, 128), 300), np.full((128, 128), 400)]
    bt_idx = np.stack([t5_bucket_np(x) for x in dists], 1)
    bt_mask = np.stack([np.where(q >= k, 0.0, -30000.0), np.zeros((128, 128)), np.zeros((128, 128)),
                        np.where(k > q, 0.0, -30000.0)], 1).astype(np.float32)
    c = (np.arange(2)[None, :, None] * 128 + np.arange(128)[:, None, None])[None]
    t = (np.arange(NT)[:, None, None, None] * 128 + np.arange(128)[None, None, None, :])
    dc = t - (16 * c + 31)
    cb_idx = t5_bucket_np(dc)
    cb_mask = np.where((dc >= 0) & (c < 255), 0.0, -30000.0).astype(np.float32)
    return bt_idx, bt_mask, cb_idx, cb_mask


def nsa_consts():
    _, bt_mask, _, cb_mask = nsa_index_tables()
    c = {'c_btmask': bt_mask, 'c_cmask': cb_mask}
    cc = np.arange(256)[:, None]
    j = np.arange(64)[None, :]
    ov = ((16 * cc < 64 * j + 64) & (16 * cc + 31 >= 64 * j) & (cc < 255)).astype(np.float32)
    c['c_ov'] = np.ascontiguousarray(ov.reshape(2, 128, 64).transpose(1, 0, 2))
    c['c_ex'] = (np.arange(S)[None, :] // 64 == np.arange(64)[:, None]).astype(np.float32)
    t = np.arange(S)[:, None]
    c['c_force'] = np.where((j == t // 64) | (j == 0), 1e9, 0.0).astype(np.float32).reshape(NT, 128, 64)
    return c


def host_consts():
    c = {}
    c['c_ident'] = np.eye(128, dtype=np.float32)
    half = 16
    inv_freq = 10000.0 ** (-np.arange(half, dtype=np.float32) / half)
    ang = np.arange(S, dtype=np.float32)[:, None] * inv_freq[None, :]
    c['c_cossin'] = np.concatenate([np.cos(ang), np.sin(ang)], axis=1).astype(np.float32)
    c.update(nsa_consts())
    kk = np.arange(128)[:, None, None] + 128 * np.arange(4)[None, :, None]
    c['c_mla_mask'] = np.where(np.arange(512)[None, None, :] >= kk, 0.0, -30000.0).astype(np.float32)
    return c


def prep_core_inputs(inp, b, consts):
    f = lambda a: np.ascontiguousarray(np.asarray(a, np.float32))
    m = dict(consts)
    m['x'] = f(inp['x'][b])
    m['ev_mix_row'] = f(np.asarray(inp['ev_mix_norm'][0]).reshape(1, D))
    m['ev_w_in'] = f(inp['ev_w_in'][0])
    m['nsa_q_col'] = f(np.asarray(inp['nsa_q_norm'][0]).reshape(64, 1))
    m['nsa_k_col'] = f(np.asarray(inp['nsa_k_norm'][0]).T)
    m['cqkv_row'] = f(np.concatenate([np.asarray(inp['mla_cq_norm'][0]), np.asarray(inp['mla_ckv_norm'][0])]).reshape(1, 384))
    m['mla_w_uq'] = f(inp['mla_w_uq'][0])
    m['mla_w_ukv'] = f(inp['mla_w_ukv'][0])
    m['mla_q_row'] = f(np.tile(np.asarray(inp['mla_q_norm'][0]), 8).reshape(1, 768))
    m['mla_k_row'] = f(np.asarray(inp['mla_k_norm'][0]).reshape(1, 96))
    m['ev_w_out'] = f(inp['ev_w_out'][0])
    m['nsa_cmp_w1'] = f(inp['nsa_cmp_w1'][0])
    m['nsa_cmp_w2'] = f(inp['nsa_cmp_w2'][0])
    m['nsa_posT'] = f(np.asarray(inp['nsa_cmp_pos'][0]).transpose(0, 2, 1))
    bt_idx, _, cb_idx, _ = nsa_index_tables()
    rb = np.asarray(inp['rel_bias'], np.float32).reshape(32, 2, 4)
    m['nsa_bt'] = f(rb[bt_idx].transpose(3, 0, 1, 4, 2))
    m['nsa_cbias'] = f(rb[cb_idx].transpose(0, 4, 1, 2, 5, 3))
    m['ev_ffn_row'] = f(np.asarray(inp['ev_ffn_norm'][0]).reshape(1, D))
    m['ffn_w_gate'] = f(inp['ffn_w_gate'][0])
    m['ffn_w_up'] = f(inp['ffn_w_up'][0])
    m['ffn_w_down'] = f(inp['ffn_w_down'][0])
    m['od_mix_row'] = f(np.asarray(inp['od_mix_norm'][0]).reshape(1, D))
    m['od_w_in'] = f(inp['od_w_in'][0])
    m['conv_col'] = f(np.asarray(inp['conv_w'][0]).reshape(3, 8, 128).transpose(2, 1, 0))
    m['od_w_out'] = f(inp['od_w_out'][0])
    m['od_ffn_row'] = f(np.asarray(inp['od_ffn_norm'][0]).reshape(1, D))
    m['moe_wrT'] = f(np.asarray(inp['moe_w_router'][0]).T.reshape(1, 8 * D))
    m['moe_b_row'] = f(np.asarray(inp['moe_b_router'][0]).reshape(1, 8))
    m['moe_w_gate'] = f(inp['moe_w_gate'][0])
    m['moe_w_up'] = f(inp['moe_w_up'][0])
    m['moe_w_down'] = f(inp['moe_w_down'][0])
    return m


def phase_mla(kb, d, nqc=8):
    with kb.phase() as ph:
        identf = ph.sb('identf', [128, 128], F32)
        ident = ph.sb('ident', [128, 128], BF16)
        kb.dma('sp', identf[:], d['c_ident'][:, :], [], [identf], identf)
        kb.op('dve', [identf], [ident], lambda en: en.tensor_copy(out=ident[:], in_=identf[:]))
        stg = ph.sb('mstg', [128, 4, 512], F32)
        Mm = ph.sb('Mm', [128, 4, 512], BF16)
        kb.dma('sp', stg[:], d['c_mla_mask'][:, :, :], [], [stg], stg)
        kb.op('dve', [stg], [Mm], lambda en: en.tensor_copy(out=Mm[:], in_=stg[:]))
        MK = ph.sb('MK', [128, 8, S], BF16)
        kb.op('dve', [], [MK], lambda en: en.memset(MK[96:128], 0.0))
        for h in range(8):
            kb.dma(('sp', 'act')[h % 2], MK[0:96, h, :], d['mla_kT'][:, h, :], [d['mla_kT_b']], [MK], MK)
        V = ph.sb('V', [128, NT, 8, 65], BF16)
        kb.op('pool', [], [V], lambda en: en.memset(V[:], 1.0))
        for kt in range(NT):
            kb.dma(('sp', 'act')[kt % 2], V[:, kt, :, 0:64], d['mla_v'][kt * 128:(kt + 1) * 128, :, :], [d['mla_v_b']], [V], V)
        MQ = ring(ph, 'MQ', [128, 8, 512], BF16)
        for b in MQ:
            kb.op('pool', [], [b], lambda en, b=b: en.memset(b[96:128], 0.0))
        Sr = [ph.ps('S%d' % i, [128, 512], F32) for i in range(2)]
        Or = [ph.ps('O%d' % i, [128, 65], F32) for i in range(4)]
        Pr = ring(ph, 'P', [128, 512], BF16, 3)
        om = ring(ph, 'om', [128, 4, 512], BF16)
        rden = ring(ph, 'rden', [128, 4], F32)
        it = 0
        for qc in range(nqc):
            MQ_ = MQ[qc % 2]
            kb.dma('sp', MQ_[0:96], d['mla_qT'][:, :, qc * 512:(qc + 1) * 512], [d['mla_qT_b']], [MQ_], MQ_)
            om_ = om[qc % 2]
            for h in range(8):
                nk = 4 * qc + 4
                slots = {}

                def emit_s(kt):
                    nonlocal it
                    S_, P_ = Sr[it % 2], Pr[it % 3]
                    it += 1
                    slots[kt] = (S_, P_)
                    c0 = max(0, kt - 4 * qc) * 128
                    diag = kt >= 4 * qc
                    kb.op('pe', [MK, MQ_], [S_], lambda en: en.matmul(S_[:, c0:512], lhsT=MK[:, h, kt * 128:(kt + 1) * 128],
                                                                     rhs=MQ_[:, h, c0:512], start=True, stop=not diag))
                    if diag:
                        kb.op('pe', [ident, Mm], [S_], lambda en: en.matmul(S_[:, c0:512], lhsT=ident[:], rhs=Mm[:, kt - 4 * qc, c0:512],
                                                                           start=False, stop=True))
                emit_s(0)
                for kt in range(nk):
                    if kt + 1 < nk:
                        emit_s(kt + 1)
                    S_, P_ = slots[kt]
                    jmin = max(0, kt - 4 * qc)
                    c0 = jmin * 128
                    kb.op('act', [S_], [P_], lambda en: en.activation(out=P_[:, c0:512], in_=S_[:, c0:512], func=AF.Exp))
                    for j in range(jmin, 4):
                        kb.op('pe', [P_, V], [Or[j]], lambda en, j=j: en.matmul(Or[j][:, :], lhsT=P_[:, j * 128:(j + 1) * 128],
                                                                               rhs=V[:, kt, h, :], start=(kt == 0), stop=(kt == 4 * qc + j)))
                rd = rden[h % 2]
                for j in range(4):
                    kb.op('dve', [Or[j]], [rd], lambda en, j=j: en.reciprocal(out=rd[:, j:j + 1], in_=Or[j][:, 64:65]))
                    kb.op('dve', [Or[j], rd], [om_], lambda en, j=j: en.tensor_scalar(out=om_[:, j, h * 64:(h + 1) * 64], in0=Or[j][:, 0:64],
                                                                                     scalar1=rd[:, j:j + 1], scalar2=None, op0=ALU.mult))
            kb.dma('sp', d['mix'][qc * 512:(qc + 1) * 512, 512:1024].rearrange('(j p) c -> p j c', p=128), om_[:],
                   [om_], [d['mix_b']], om_)


def make_ident(kb, ph, d):
    identf = ph.sb('identf', [128, 128], F32)
    ident = ph.sb('ident', [128, 128], BF16)
    kb.dma('sp', identf[:], d['c_ident'][:, :], [], [identf], identf)
    kb.op('dve', [identf], [ident], lambda en: en.tensor_copy(out=ident[:], in_=identf[:]))
    return ident


def norm_transpose(kb, x_, ssq_, rstd_, junk, xs_, T0, ident, dstT, cols):
    kb.op('pool', [], [ssq_], lambda en: en.memset(ssq_[:, 0:1], 0.0))
    kb.op('act', [x_], [junk, ssq_], lambda en: en.activation(out=junk[:], in_=x_[:], func=AF.Square, accum_out=ssq_[:, 0:1]))
    kb.op('act', [ssq_], [rstd_], lambda en: en.activation(out=rstd_[:, 0:1], in_=ssq_[:, 0:1], func=AF.Sqrt, bias=EPS, scale=1.0 / D))
    kb.op('dve', [rstd_], [rstd_], lambda en: en.reciprocal(out=rstd_[:, 0:1], in_=rstd_[:, 0:1]))
    kb.op('dve', [x_, rstd_], [xs_], lambda en: en.tensor_scalar(out=xs_[:], in0=x_[:], scalar1=rstd_[:, 0:1], scalar2=None, op0=ALU.mult))
    for k in range(8):
        kb.op('pe', [xs_, ident], [T0], lambda en, k=k: en.transpose(out=T0[:, k, :], in_=xs_[:, k * 128:(k + 1) * 128], identity=ident[:]))
    kb.op('act', [T0], [dstT], lambda en: en.copy(out=dstT[:, 0:8, cols], in_=T0[:]))


def phase_wout(kb, d, nt=NT):
    with kb.phase() as ph:
        ident = make_ident(kb, ph, d)
        Wo = ph.sb('Wo', [128, 8, D], BF16)
        load_w(kb, ph, Wo, d['ev_w_out'], D, D)
        T0 = ph.ps('T0', [128, 8, 128], BF16)
        M = [ph.ps('M%d' % i, [128, 512], F32) for i in range(4)]
        mx = ring(ph, 'mx', [128, D], BF16)
        xr = ring(ph, 'xr', [128, D], F32)
        mT = ring(ph, 'mT', [128, 8, 128], BF16)
        xo = ring(ph, 'xo', [128, D], F32)
        for tt in range(nt):
            r = tt % 2
            rows = slice(tt * 128, (tt + 1) * 128)
            kb.dma('sp', mx[r][:], d['mix'][rows, :], [d['mix_b']], [mx[r]], mx[r])
            kb.dma('act', xr[r][:], d['x'][rows, :], [], [xr[r]], xr[r])
            for k in range(8):
                kb.op('pe', [mx[r], ident], [T0], lambda en, k=k: en.transpose(out=T0[:, k, :], in_=mx[r][:, k * 128:(k + 1) * 128], identity=ident[:]))
            kb.op('act', [T0], [mT[r]], lambda en: en.copy(out=mT[r][:], in_=T0[:]))
            for c in range(2):
                Mc = M[(tt * 2 + c) % 4]
                for k in range(8):
                    kb.op('pe', [mT[r], Wo], [Mc], lambda en, k=k, c=c: en.matmul(Mc[:, :], lhsT=mT[r][:, k, :], rhs=Wo[:, k, c * 512:(c + 1) * 512],
                                                                                 start=(k == 0), stop=(k == 7)))
                kb.op('dve', [Mc, xr[r]], [xo[r]], lambda en, c=c: en.tensor_tensor(out=xo[r][:, c * 512:(c + 1) * 512], in0=Mc[:, :],
                                                                                   in1=xr[r][:, c * 512:(c + 1) * 512], op=ALU.add))
            kb.dma('sp', d['x1a'][rows, :], xo[r][:], [xo[r]], [d['x1a_b']], xo[r])


def load_w_thunks(kb, dst, src, K, N):
    return [lambda kc=kc: kb.dma('pool', dst[:, kc, :], src[kc * 128:(kc + 1) * 128, :], [], [dst], dst) for kc in range(K // 128)]


def expert_passes(kb, d, passes, nst=8):
    with kb.phase() as ph:
        ident = make_ident(kb, ph, d)
        WS = [dict(Wg=ph.sb('Wg%d' % i, [128, 8, DFE], BF16), Wu=ph.sb('Wu%d' % i, [128, 8, DFE], BF16),
                   Wd=ph.sb('Wd%d' % i, [128, 11, D], BF16)) for i in range(2)]
        grow = ph.sb('grow', [128, D], F32)
        kb.dma('sp', grow[:], bc_row(passes[0]['grow'], D), [], [grow], grow)
        T0 = ph.ps('T0', [128, 8, 128], BF16)
        G = [ph.ps('G%d' % i, [128, 512], F32) for i in range(2)]
        U = [ph.ps('U%d' % i, [128, 512], F32) for i in range(2)]
        Y = [ph.ps('Y%d' % i, [128, 512], F32) for i in range(2)]
        xr = ring(ph, 'xr', [128, D], F32)
        rs = ring(ph, 'rs', [128, D], F32)
        gt = ring(ph, 'gt', [128, 8], F32)
        junk = ph.sb('junk', [128, D], BF16)
        ssq = ring(ph, 'ssq', [128, 1], F32)
        rstd = ring(ph, 'rstd', [128, 1], F32)
        xs = ring(ph, 'xs', [128, D], BF16)
        xnT = ring(ph, 'xnT', [128, 8, 512], BF16)
        sg = ring(ph, 'sg', [128, 512], F32)
        hT = ph.sb('hT', [128, 11, 512], BF16)
        ctr = [0]

        def weight_thunks(p, ws):
            th = load_w_thunks(kb, ws['Wg'], p['wg'], D, DFE)
            th += load_w_thunks(kb, ws['Wu'], p['wu'], D, DFE)
            th += load_w_thunks(kb, ws['Wd'], p['wd'], DFE, D)
            return th

        for t in weight_thunks(passes[0], WS[0]):
            t()
        it = 0
        for pi, p in enumerate(passes):
            ws = WS[pi % 2]
            Wg, Wu, Wd = ws['Wg'], ws['Wu'], ws['Wd']
            pending = weight_thunks(passes[pi + 1], WS[(pi + 1) % 2]) if pi + 1 < len(passes) else []
            per_st = (len(pending) + nst - 1) // nst
            src, resid, dst = d[p['src']], d[p['resid']], d[p['dst']]
            srcb = [d[p['src'] + '_b']] if p['src'] + '_b' in d else []
            resb = [d[p['resid'] + '_b']] if p['resid'] + '_b' in d else []

            def prep_a(st, j):
                tt = st * 4 + j
                r = tt % 2
                rows = slice(tt * 128, (tt + 1) * 128)
                x_, ssq_, rstd_, xs_ = xr[r], ssq[r], rstd[r], xs[r]
                kb.dma('sp', x_[:], src[rows, :], srcb, [x_], x_)
                kb.op('pool', [], [ssq_], lambda en: en.memset(ssq_[:, 0:1], 0.0))
                kb.op('act', [x_], [junk, ssq_], lambda en: en.activation(out=junk[:], in_=x_[:], func=AF.Square, accum_out=ssq_[:, 0:1]))
                kb.op('act', [ssq_], [rstd_], lambda en: en.activation(out=rstd_[:, 0:1], in_=ssq_[:, 0:1], func=AF.Sqrt, bias=EPS, scale=1.0 / D))
                kb.op('dve', [rstd_], [rstd_], lambda en: en.reciprocal(out=rstd_[:, 0:1], in_=rstd_[:, 0:1]))
                kb.op('dve', [x_, rstd_, grow], [xs_], lambda en: en.scalar_tensor_tensor(out=xs_[:], in0=x_[:], scalar=rstd_[:, 0:1], in1=grow[:],
                                                                                         op0=ALU.mult, op1=ALU.mult))

            def prep_b(st, j):
                tt = st * 4 + j
                xs_ = xs[tt % 2]
                xnT_ = xnT[st % 2]
                for k in range(8):
                    kb.op('pe', [xs_, ident], [T0], lambda en, k=k: en.transpose(out=T0[:, k, :], in_=xs_[:, k * 128:(k + 1) * 128], identity=ident[:]))
                kb.op('act', [T0], [xnT_], lambda en: en.copy(out=xnT_[:, 0:8, j * 128:(j + 1) * 128], in_=T0[:]))

            if pi == 0:
                for j in range(4):
                    prep_a(0, j)
                    prep_b(0, j)
            assert all(q['src'] == p['src'] for q in passes)
            for st in range(nst):
                xnT_ = xnT[st % 2]
                nxt = (st + 1 < nst) or (pi + 1 < len(passes) and nst % 2 == 0)
                st1 = (st + 1) % nst
                for f in range(11):
                    G_, U_ = G[f % 2], U[f % 2]
                    for k in range(8):
                        kb.op('pe', [Wg, xnT_], [G_], lambda en, k=k: en.matmul(G_[:, :], lhsT=Wg[:, k, f * 128:(f + 1) * 128], rhs=xnT_[:, k, :],
                                                                               start=(k == 0), stop=(k == 7)))
                    for k in range(8):
                        kb.op('pe', [Wu, xnT_], [U_], lambda en, k=k: en.matmul(U_[:, :], lhsT=Wu[:, k, f * 128:(f + 1) * 128], rhs=xnT_[:, k, :],
                                                                               start=(k == 0), stop=(k == 7)))
                    sg_ = sg[f % 2]
                    kb.op('act', [G_], [sg_], lambda en: en.activation(out=sg_[:], in_=G_[:, :], func=AF.Silu))
                    kb.op('dve', [sg_, U_], [hT], lambda en: en.tensor_tensor(out=hT[:, f, :], in0=U_[:, :], in1=sg_[:], op=ALU.mult))
                    if nxt and f in (2, 4, 6, 8):
                        prep_b(st1, f // 2 - 1)
                    if nxt and f in (0, 2, 4, 6):
                        prep_a(st1, f // 2)
                for j in range(4):
                    tt = st * 4 + j
                    r = tt % 2
                    rows = slice(tt * 128, (tt + 1) * 128)
                    kb.dma('sp', rs[r][:], resid[rows, :], resb, [rs[r]], rs[r])
                    if p['gate'] is not None:
                        kb.dma('sp', gt[r][:], d[p['gate'][0]][rows, :], [d[p['gate'][0] + '_b']], [gt[r]], gt[r])
                    for c in range(2):
                        Y_ = Y[it % 2]
                        it += 1
                        for f in range(11):
                            kb.op('pe', [hT, Wd], [Y_], lambda en, f=f, c=c: en.matmul(Y_[:, :], lhsT=hT[:, f, j * 128:(j + 1) * 128],
                                                                                      rhs=Wd[:, f, c * 512:(c + 1) * 512], start=(f == 0), stop=(f == 10)))
                        cs_ = slice(c * 512, (c + 1) * 512)
                        if p['gate'] is not None:
                            e = p['gate'][1]
                            kb.op('dve', [Y_, rs[r], gt[r]], [rs[r]], lambda en: en.scalar_tensor_tensor(
                                out=rs[r][:, cs_], in0=Y_[:, :], scalar=gt[r][:, e:e + 1], in1=rs[r][:, cs_], op0=ALU.mult, op1=ALU.add))
                        else:
                            kb.op('dve', [Y_, rs[r]], [rs[r]], lambda en: en.tensor_tensor(out=rs[r][:, cs_], in0=Y_[:, :], in1=rs[r][:, cs_], op=ALU.add))
                    kb.dma('sp', dst[rows, :], rs[r][:], [rs[r]], [d[p['dst'] + '_b']], rs[r])
                for t in pending[st * per_st:(st + 1) * per_st]:
                    t()


def phase_odd_mixer(kb, d, nst=8, fuse_router=True):
    with kb.phase() as ph:
        ident = make_ident(kb, ph, d)
        Wi = ph.sb('Wi', [128, 8, 3 * D], BF16)
        Wo = ph.sb('Wo', [128, 8, D], BF16)
        cw = ph.sb('cw', [128, 8, 3], F32)
        grow = ph.sb('grow', [128, D], F32)
        kb.dma('sp', grow[:], bc_row(d['od_mix_row'], D), [], [grow], grow)
        kb.dma('sp', cw[:], d['conv_col'][:, :, :], [], [cw], cw)
        load_w(kb, ph, Wi, d['od_w_in'], D, 3 * D)
        load_w(kb, ph, Wo, d['od_w_out'], D, D)
        carry = ph.sb('carry', [128, 8, 2], F32)
        kb.op('pool', [], [carry], lambda en: en.memset(carry[:], 0.0))
        if fuse_router:
            wr = ph.sb('wr', [128, 8, D], F32)
            grow2 = ph.sb('grow2', [128, D], F32)
            brow = ph.sb('brow', [128, 8], F32)
            kb.dma('sp', wr[:], bass.AP(d['moe_wrT'], 0, [[0, 128], [1, 8 * D]]), [], [wr], wr)
            kb.dma('sp', grow2[:], bc_row(d['od_ffn_row'], D), [], [grow2], grow2)
            kb.dma('sp', brow[:], bc_row(d['moe_b_row'], 8), [], [brow], brow)
            kb.op('dve', [wr, grow2], [wr], lambda en: en.tensor_tensor(out=wr[:], in0=wr[:], in1=grow2[:].unsqueeze(1).broadcast_to([128, 8, D]), op=ALU.mult))
            xn2 = ring(ph, 'xn2', [128, D], F32)
            jk2 = ring(ph, 'jk2', [128, D], BF16)
            ssq2 = ring(ph, 'ssq2', [128, 1], F32)
            rstd2 = ring(ph, 'rstd2', [128, 1], F32)
            lg = ring(ph, 'lg', [128, 8], F32)
            m8 = ring(ph, 'm8', [128, 8], F32)
            ww = ring(ph, 'ww', [128, 4], F32)
            g1 = ring(ph, 'g1', [128, 8], F32)
            g2 = ring(ph, 'g2', [128, 8], F32)
        T0 = ph.ps('T0', [128, 8, 128], BF16)
        Bp = [ph.ps('Bp%d' % i, [128, 512], F32) for i in range(2)]
        Cp = ph.ps('Cp', [128, 512], F32)
        Up = ph.ps('Up', [128, 512], F32)
        Y = [ph.ps('Y%d' % i, [128, 512], F32) for i in range(2)]
        xr = ring(ph, 'xr', [128, D], F32)
        junk = ph.sb('junk', [128, D], BF16)
        ssq = ring(ph, 'ssq', [128, 1], F32)
        rstd = ring(ph, 'rstd', [128, 1], F32)
        xs = ring(ph, 'xs', [128, D], BF16)
        xnT = ring(ph, 'xnT', [128, 8, 512], BF16)
        csb = ring(ph, 'csb', [128, 512], F32)
        cu = ring(ph, 'cu', [128, 514], F32)
        yy = ring(ph, 'yy', [128, 512], F32)
        mT = ring(ph, 'mT', [128, 8, 512], BF16)
        xo = ring(ph, 'xo', [128, D], F32)
        def prep_a(st, j):
            tt = st * 4 + j
            r = tt % 2
            rows = slice(tt * 128, (tt + 1) * 128)
            x_ = xr[r]
            kb.dma('sp', x_[:], d['x1'][rows, :], [d['x1_b']], [x_], x_)
            kb.op('pool', [], [ssq[r]], lambda en: en.memset(ssq[r][:, 0:1], 0.0))
            kb.op('act', [x_], [junk, ssq[r]], lambda en: en.activation(out=junk[:], in_=x_[:], func=AF.Square, accum_out=ssq[r][:, 0:1]))
            kb.op('act', [ssq[r]], [rstd[r]], lambda en: en.activation(out=rstd[r][:, 0:1], in_=ssq[r][:, 0:1], func=AF.Sqrt, bias=EPS, scale=1.0 / D))
            kb.op('dve', [rstd[r]], [rstd[r]], lambda en: en.reciprocal(out=rstd[r][:, 0:1], in_=rstd[r][:, 0:1]))
            kb.op('dve', [x_, rstd[r], grow], [xs[r]], lambda en: en.scalar_tensor_tensor(out=xs[r][:], in0=x_[:], scalar=rstd[r][:, 0:1], in1=grow[:],
                                                                                        op0=ALU.mult, op1=ALU.mult))

        def prep_b(st, j):
            tt = st * 4 + j
            r = tt % 2
            xnT_ = xnT[st % 2]
            for k in range(8):
                kb.op('pe', [xs[r], ident], [T0], lambda en, k=k: en.transpose(out=T0[:, k, :], in_=xs[r][:, k * 128:(k + 1) * 128], identity=ident[:]))
            kb.op('act', [T0], [xnT_], lambda en: en.copy(out=xnT_[:, :, j * 128:(j + 1) * 128], in_=T0[:]))

        it = 0
        for st in range(nst):
            xnT_, mT_ = xnT[st % 2], mT[st % 2]
            if st == 0:
                for j in range(4):
                    prep_a(0, j)
                    prep_b(0, j)
            nxt = st + 1 < nst
            for cc in range(8):
                Bp_ = Bp[cc % 2]
                for (P_, off) in ((Bp_, 0), (Cp, D), (Up, 2 * D)):
                    for k in range(8):
                        kb.op('pe', [Wi, xnT_], [P_], lambda en, k=k: en.matmul(P_[:, :], lhsT=Wi[:, k, off + cc * 128:off + (cc + 1) * 128], rhs=xnT_[:, k, :],
                                                                               start=(k == 0), stop=(k == 7)))
                csb_, cu_, yy_ = csb[cc % 2], cu[cc % 2], yy[cc % 2]
                kb.op('act', [Cp], [csb_], lambda en: en.copy(out=csb_[:], in_=Cp[:, :]))
                kb.op('dve', [csb_, Up], [cu_], lambda en: en.tensor_tensor(out=cu_[:, 2:514], in0=Up[:, :], in1=csb_[:], op=ALU.mult))
                kb.op('pool', [carry], [cu_], lambda en: en.tensor_copy(out=cu_[:, 0:2], in_=carry[:, cc, :]))
                kb.op('pool', [cu_], [carry], lambda en: en.tensor_copy(out=carry[:, cc, :], in_=cu_[:, 512:514]))
                kb.op('dve', [cu_, cw], [yy_], lambda en: en.tensor_scalar(out=yy_[:], in0=cu_[:, 0:512], scalar1=cw[:, cc, 0:1], scalar2=None, op0=ALU.mult))
                kb.op('dve', [cu_, cw, yy_], [yy_], lambda en: en.scalar_tensor_tensor(out=yy_[:], in0=cu_[:, 1:513], scalar=cw[:, cc, 1:2], in1=yy_[:],
                                                                                     op0=ALU.mult, op1=ALU.add))
                kb.op('dve', [cu_, cw, yy_], [yy_], lambda en: en.scalar_tensor_tensor(out=yy_[:], in0=cu_[:, 2:514], scalar=cw[:, cc, 2:3], in1=yy_[:],
                                                                                     op0=ALU.mult, op1=ALU.add))
                kb.op('dve', [yy_, Bp_], [mT_], lambda en: en.tensor_tensor(out=mT_[:, cc, :], in0=Bp_[:, :], in1=yy_[:], op=ALU.mult))
                if nxt and cc in (1, 3, 5, 7):
                    prep_b(st + 1, (cc - 1) // 2)
                if nxt and cc in (0, 2, 4, 6):
                    prep_a(st + 1, cc // 2)
            for j in range(4):
                tt = st * 4 + j
                r = tt % 2
                rows = slice(tt * 128, (tt + 1) * 128)
                kb.dma('sp', xo[r][:], d['x1'][rows, :], [d['x1_b']], [xo[r]], xo[r])
                for c in range(2):
                    Y_ = Y[it % 2]
                    it += 1
                    for cc in range(8):
                        kb.op('pe', [mT_, Wo], [Y_], lambda en, cc=cc: en.matmul(Y_[:, :], lhsT=mT_[:, cc, j * 128:(j + 1) * 128], rhs=Wo[:, cc, c * 512:(c + 1) * 512],
                                                                                start=(cc == 0), stop=(cc == 7)))
                    kb.op('dve', [Y_, xo[r]], [xo[r]], lambda en: en.tensor_tensor(out=xo[r][:, c * 512:(c + 1) * 512], in0=Y_[:, :],
                                                                                  in1=xo[r][:, c * 512:(c + 1) * 512], op=ALU.add))
                kb.dma('sp', d['x2a'][rows, :], xo[r][:], [xo[r]], [d['x2a_b']], xo[r])
                if fuse_router:
                    x_, lg_, m8_, ww_ = xo[r], lg[r], m8[r], ww[r]
                    kb.op('pool', [], [ssq2[r]], lambda en: en.memset(ssq2[r][:, 0:1], 0.0))
                    kb.op('pool', [], [lg_], lambda en: en.memset(lg_[:], 0.0))
                    kb.op('act', [x_], [jk2[0], ssq2[r]], lambda en: en.activation(out=jk2[0][:], in_=x_[:], func=AF.Square, accum_out=ssq2[r][:, 0:1]))
                    kb.op('act', [ssq2[r]], [rstd2[r]], lambda en: en.activation(out=rstd2[r][:, 0:1], in_=ssq2[r][:, 0:1], func=AF.Sqrt, bias=EPS, scale=1.0 / D))
                    kb.op('dve', [rstd2[r]], [rstd2[r]], lambda en: en.reciprocal(out=rstd2[r][:, 0:1], in_=rstd2[r][:, 0:1]))
                    kb.op('act', [x_, rstd2[r]], [xn2[r]], lambda en: en.activation(out=xn2[r][:], in_=x_[:], func=AF.Copy, scale=rstd2[r][:, 0:1]))
                    for e in range(8):
                        jk = jk2[e % 2]
                        kb.op('dve', [xn2[r], wr, lg_], [jk, lg_], lambda en: en.scalar_tensor_tensor(out=jk[:], in0=xn2[r][:], scalar=1.0, in1=wr[:, e, :],
                                                                                                    op0=ALU.mult, op1=ALU.mult, accum_out=lg_[:, e:e + 1]))
                    kb.op('dve', [lg_, brow], [lg_], lambda en: en.tensor_tensor(out=lg_[:], in0=lg_[:], in1=brow[:], op=ALU.add))
                    kb.op('dve', [lg_], [m8_], lambda en: en.max(out=m8_[:], in_=lg_[:]))
                    kb.op('dve', [m8_], [ww_], lambda en: en.tensor_tensor(out=ww_[:, 0:1], in0=m8_[:, 1:2], in1=m8_[:, 0:1], op=ALU.subtract))
                    kb.op('act', [ww_], [ww_], lambda en: en.activation(out=ww_[:, 1:2], in_=ww_[:, 0:1], func=AF.Exp))
                    kb.op('dve', [ww_], [ww_], lambda en: en.tensor_scalar(out=ww_[:, 2:3], in0=ww_[:, 1:2], scalar1=1.0, scalar2=None, op0=ALU.add))
                    kb.op('dve', [ww_], [ww_], lambda en: en.reciprocal(out=ww_[:, 2:3], in_=ww_[:, 2:3]))
                    kb.op('dve', [ww_], [ww_], lambda en: en.tensor_tensor(out=ww_[:, 3:4], in0=ww_[:, 1:2], in1=ww_[:, 2:3], op=ALU.mult))
                    kb.op('dve', [lg_, m8_, ww_], [g1[r]], lambda en: en.tensor_scalar(out=g1[r][:], in0=lg_[:], scalar1=m8_[:, 0:1], scalar2=ww_[:, 2:3],
                                                                                      op0=ALU.is_equal, op1=ALU.mult))
                    kb.op('dve', [lg_, m8_, ww_], [g2[r]], lambda en: en.tensor_scalar(out=g2[r][:], in0=lg_[:], scalar1=m8_[:, 1:2], scalar2=ww_[:, 3:4],
                                                                                      op0=ALU.is_equal, op1=ALU.mult))
                    kb.op('dve', [g1[r], g2[r]], [g1[r]], lambda en: en.tensor_tensor(out=g1[r][:], in0=g1[r][:], in1=g2[r][:], op=ALU.add))
                    kb.dma('sp', d['moe_gate'][rows, :], g1[r][:], [g1[r]], [d['moe_gate_b']], g1[r])


def phase_router(kb, d, nt=NT):
    with kb.phase() as ph:
        wr = ph.sb('wr', [128, 8, D], F32)
        grow = ph.sb('grow', [128, D], F32)
        brow = ph.sb('brow', [128, 8], F32)
        kb.dma('sp', wr[:], bass.AP(d['moe_wrT'], 0, [[0, 128], [1, 8 * D]]), [], [wr], wr)
        kb.dma('sp', grow[:], bc_row(d['od_ffn_row'], D), [], [grow], grow)
        kb.dma('sp', brow[:], bc_row(d['moe_b_row'], 8), [], [brow], brow)
        kb.op('dve', [wr, grow], [wr], lambda en: en.tensor_tensor(out=wr[:], in0=wr[:], in1=grow[:].unsqueeze(1).broadcast_to([128, 8, D]), op=ALU.mult))
        xr = ring(ph, 'xr', [128, D], F32)
        xn = ring(ph, 'xn', [128, D], F32)
        junk = ring(ph, 'junk', [128, D], F32)
        ssq = ring(ph, 'ssq', [128, 1], F32)
        rstd = ring(ph, 'rstd', [128, 1], F32)
        lg = ring(ph, 'lg', [128, 8], F32)
        m8 = ring(ph, 'm8', [128, 8], F32)
        ww = ring(ph, 'ww', [128, 4], F32)
        g1 = ring(ph, 'g1', [128, 8], F32)
        g2 = ring(ph, 'g2', [128, 8], F32)
        for tt in range(nt):
            r = tt % 2
            rows = slice(tt * 128, (tt + 1) * 128)
            x_, lg_, m8_, ww_ = xr[r], lg[r], m8[r], ww[r]
            kb.dma('sp', x_[:], d['x2a'][rows, :], [d['x2a_b']], [x_], x_)
            kb.op('pool', [], [ssq[r]], lambda en: en.memset(ssq[r][:, 0:1], 0.0))
            kb.op('act', [x_], [junk[0], ssq[r]], lambda en: en.activation(out=junk[0][:], in_=x_[:], func=AF.Square, accum_out=ssq[r][:, 0:1]))
            kb.op('act', [ssq[r]], [rstd[r]], lambda en: en.activation(out=rstd[r][:, 0:1], in_=ssq[r][:, 0:1], func=AF.Sqrt, bias=EPS, scale=1.0 / D))
            kb.op('dve', [rstd[r]], [rstd[r]], lambda en: en.reciprocal(out=rstd[r][:, 0:1], in_=rstd[r][:, 0:1]))
            kb.op('act', [x_, rstd[r]], [xn[r]], lambda en: en.activation(out=xn[r][:], in_=x_[:], func=AF.Copy, scale=rstd[r][:, 0:1]))
            kb.op('pool', [], [lg_], lambda en: en.memset(lg_[:], 0.0))
            for e in range(8):
                jk = junk[e % 2]
                kb.op('dve', [xn[r], wr, lg_], [jk, lg_], lambda en: en.scalar_tensor_tensor(out=jk[:], in0=xn[r][:], scalar=1.0, in1=wr[:, e, :],
                                                                                             op0=ALU.mult, op1=ALU.mult, accum_out=lg_[:, e:e + 1]))
            kb.op('dve', [lg_, brow], [lg_], lambda en: en.tensor_tensor(out=lg_[:], in0=lg_[:], in1=brow[:], op=ALU.add))
            kb.op('dve', [lg_], [m8_], lambda en: en.max(out=m8_[:], in_=lg_[:]))
            kb.op('dve', [m8_], [ww_], lambda en: en.tensor_tensor(out=ww_[:, 0:1], in0=m8_[:, 1:2], in1=m8_[:, 0:1], op=ALU.subtract))
            kb.op('act', [ww_], [ww_], lambda en: en.activation(out=ww_[:, 1:2], in_=ww_[:, 0:1], func=AF.Exp))
            kb.op('dve', [ww_], [ww_], lambda en: en.tensor_scalar(out=ww_[:, 2:3], in0=ww_[:, 1:2], scalar1=1.0, scalar2=None, op0=ALU.add))
            kb.op('dve', [ww_], [ww_], lambda en: en.reciprocal(out=ww_[:, 2:3], in_=ww_[:, 2:3]))
            kb.op('dve', [ww_], [ww_], lambda en: en.tensor_tensor(out=ww_[:, 3:4], in0=ww_[:, 1:2], in1=ww_[:, 2:3], op=ALU.mult))
            kb.op('dve', [lg_, m8_, ww_], [g1[r]], lambda en: en.tensor_scalar(out=g1[r][:], in0=lg_[:], scalar1=m8_[:, 0:1], scalar2=ww_[:, 2:3],
                                                                              op0=ALU.is_equal, op1=ALU.mult))
            kb.op('dve', [lg_, m8_, ww_], [g2[r]], lambda en: en.tensor_scalar(out=g2[r][:], in0=lg_[:], scalar1=m8_[:, 1:2], scalar2=ww_[:, 3:4],
                                                                              op0=ALU.is_equal, op1=ALU.mult))
            kb.op('dve', [g1[r], g2[r]], [g1[r]], lambda en: en.tensor_tensor(out=g1[r][:], in0=g1[r][:], in1=g2[r][:], op=ALU.add))
            kb.dma('sp', d['moe_gate'][rows, :], g1[r][:], [g1[r]], [d['moe_gate_b']], g1[r])


def phase_cmp(kb, d):
    with kb.phase() as ph:
        ident = make_ident(kb, ph, d)
        kcol = ph.sb('kcol', [64, 3], F32)
        kb.dma('sp', kcol[:], d['nsa_k_col'][:, :], [], [kcol], kcol)
        stg = ph.sb('w1s', [128, 32, 256], F32)
        W1 = ph.sb('W1', [128, 32, 256], BF16)
        w2s = ph.sb('w2s', [128, 2, 64], F32)
        W2 = ph.sb('W2', [128, 2, 64], BF16)
        pTs = ph.sb('pTs', [64, 32], F32)
        pT = ph.sb('pT', [64, 32], BF16)
        cT = ph.sb('cT', [128, S], BF16)
        b1 = ph.sb('b1', [128, 2], F32)
        g1T = ph.sb('g1T', [128, 2, 256], BF16)
        xx = ring(ph, 'xx', [128, 255], F32)
        x2 = ring(ph, 'x2', [128, 255], F32)
        th = ring(ph, 'th', [128, 255], F32)
        kcT = ph.sb('kcT', [64, 2, 256], BF16)
        vc = ph.sb('vc', [128, 2, 2, 64], BF16)
        kn = ring(ph, 'kn', [128, 64], BF16)
        junk = ph.sb('junk', [128, 64], F32)
        ssq = ring(ph, 'ssq', [128, 1], F32)
        rstd = ring(ph, 'rstd', [128, 1], F32)
        kb.op('pool', [], [kcT], lambda en: en.memset(kcT[:], 0.0))
        kb.op('pool', [], [vc], lambda en: en.memset(vc[:], 0.0))
        kb.op('pool', [], [g1T], lambda en: en.memset(g1T[:], 0.0))
        H1 = [ph.ps('H%d' % i, [128, 256], F32) for i in range(2)]
        B1 = ph.ps('B1', [128, 2], F32)
        O2 = [ph.ps('O2%d' % i, [128, 64], F32) for i in range(2)]
        T0 = ph.ps('T0', [128, 8, 128], BF16)
        for kv in range(2):
            w1v = d['nsa_cmp_w1'][kv].rearrange('(l dd) h -> dd l h', dd=64)
            kb.dma('sp', stg[0:64], w1v, [], [stg], stg)
            kb.dma('act', stg[64:128], w1v, [], [stg], stg)
            kb.op('dve', [stg], [W1], lambda en: en.tensor_copy(out=W1[:], in_=stg[:]))
            kb.dma('sp', w2s[:], d['nsa_cmp_w2'][kv].rearrange('(c p) o -> p c o', p=128), [], [w2s], w2s)
            kb.op('dve', [w2s], [W2], lambda en: en.tensor_copy(out=W2[:], in_=w2s[:]))
            kb.dma('sp', pTs[:], d['nsa_posT'][kv], [], [pTs], pTs)
            kb.op('dve', [pTs], [pT], lambda en: en.tensor_copy(out=pT[:], in_=pTs[:]))
            kb.dma('sp', cT[:], d['nsa_cT'][:, kv, :], [d['nsa_cT_b']], [cT], cT)
            for hc in range(2):
                for l in range(32):
                    kb.op('pe', [W1, pT], [B1], lambda en, l=l: en.matmul(B1[:, hc:hc + 1], lhsT=W1[0:64, l, hc * 128:(hc + 1) * 128], rhs=pT[:, l:l + 1],
                                                                         start=(l == 0), stop=(l == 31)))
            kb.op('act', [B1], [b1], lambda en: en.copy(out=b1[:], in_=B1[:, :]))
            for g in range(2):
                gs = slice(g * 64, (g + 1) * 64)
                for hc in range(2):
                    H_ = H1[hc]
                    for l in range(32):
                        kb.op('pe', [W1, cT], [H_], lambda en, l=l: en.matmul(H_[:, 0:255], lhsT=W1[gs, l, hc * 128:(hc + 1) * 128],
                                                                             rhs=cT[gs, l:l + 16 * 254 + 1:16], start=(l == 0), stop=(l == 31)))
                    xx_, x2_, th_ = xx[hc], x2[hc], th[hc]
                    kb.op('act', [H_, b1], [xx_], lambda en: en.activation(out=xx_[:], in_=H_[:, 0:255], func=AF.Identity, bias=b1[:, hc:hc + 1]))
                    kb.op('act', [xx_], [x2_], lambda en: en.activation(out=x2_[:], in_=xx_[:], func=AF.Square))
                    kb.op('dve', [x2_], [x2_], lambda en: en.tensor_scalar(out=x2_[:], in0=x2_[:], scalar1=0.044715, scalar2=1.0, op0=ALU.mult, op1=ALU.add))
                    kb.op('dve', [x2_, xx_], [x2_], lambda en: en.tensor_tensor(out=x2_[:], in0=x2_[:], in1=xx_[:], op=ALU.mult))
                    kb.op('act', [x2_], [th_], lambda en: en.activation(out=th_[:], in_=x2_[:], func=AF.Tanh, scale=0.7978845608028654))
                    kb.op('dve', [th_], [th_], lambda en: en.tensor_scalar(out=th_[:], in0=th_[:], scalar1=1.0, scalar2=0.5, op0=ALU.add, op1=ALU.mult))
                    kb.op('dve', [th_, xx_], [g1T], lambda en: en.tensor_tensor(out=g1T[:, hc, 0:255], in0=th_[:], in1=xx_[:], op=ALU.mult))
                for ct in range(2):
                    O_ = O2[ct]
                    for hc in range(2):
                        kb.op('pe', [g1T, W2], [O_], lambda en, hc=hc: en.matmul(O_[:, :], lhsT=g1T[:, hc, ct * 128:(ct + 1) * 128], rhs=W2[:, hc, :],
                                                                                start=(hc == 0), stop=(hc == 1)))
                    if kv == 1:
                        kb.op('act', [O_], [vc], lambda en: en.copy(out=vc[:, ct, g, :], in_=O_[:, :]))
                    else:
                        r = ct
                        kb.op('pool', [], [ssq[r]], lambda en: en.memset(ssq[r][:], 0.0))
                        kb.op('act', [O_], [junk, ssq[r]], lambda en: en.activation(out=junk[:], in_=O_[:, :], func=AF.Square, accum_out=ssq[r][:, 0:1]))
                        kb.op('act', [ssq[r]], [rstd[r]], lambda en: en.activation(out=rstd[r][:], in_=ssq[r][:], func=AF.Sqrt, bias=EPS, scale=1.0 / 64))
                        kb.op('dve', [rstd[r]], [rstd[r]], lambda en: en.reciprocal(out=rstd[r][:], in_=rstd[r][:]))
                        kb.op('dve', [O_, rstd[r]], [kn[r]], lambda en: en.tensor_scalar(out=kn[r][:], in0=O_[:, :], scalar1=rstd[r][:, 0:1], scalar2=None, op0=ALU.mult))
                        kb.op('pe', [kn[r], ident], [T0], lambda en: en.transpose(out=T0[0:64, ct, :], in_=kn[r][:], identity=ident[:]))
                        n = 128 if ct == 0 else 127
                        kb.op('act', [T0, kcol], [kcT], lambda en: en.activation(out=kcT[:, g, ct * 128:ct * 128 + n], in_=T0[0:64, ct, 0:n], func=AF.Copy,
                                                                                 scale=kcol[:, 0:1]))
        kb.dma('sp', d['nsa_kcT'][:, :, :], kcT[:], [kcT], [d['nsa_kcT_b']], kcT)
        kb.dma('sp', d['nsa_vc'][:, :, :, :], vc[:], [vc], [d['nsa_vc_b']], vc)


def phase_nsa(kb, d, qts=None, groups=(0, 1)):
    qts = list(range(NT)) if qts is None else qts
    with kb.phase() as ph:
        ident = make_ident(kb, ph, d)
        QT = ph.sb('QT', [128, 4, S], BF16)
        KsT = ph.sb('KsT', [128, S], BF16)
        KwT = ph.sb('KwT', [128, S], BF16)
        kcT = ph.sb('kcT', [128, 256], BF16)
        Vs = ph.sb('Vs', [128, NT, 65], BF16)
        Vw = ph.sb('Vw', [128, NT, 65], BF16)
        Vc = ph.sb('Vc', [128, 2, 129], BF16)
        ovs = ph.sb('ovs', [128, 2, 64], F32)
        exs = ph.sb('exs', [64, 1024], F32)
        Ex = ph.sb('Ex', [128, S], BF16)
        bts = ph.sb('bts', [128, 4, 4, 128], F32)
        btm = ph.sb('btm', [128, 4, 128], F32)
        Bt = ph.sb('Bt', [128, 4, 512], BF16)
        cbs = ring(ph, 'cbs', [128, 2, 4, 128], F32)
        cbm = ring(ph, 'cbm', [128, 2, 128], F32)
        CB = ring(ph, 'CB', [128, 2, 512], BF16)
        frc = ring(ph, 'frc', [128, 64], F32)
        gt = ring(ph, 'gt', [128, 24], F32)
        Pr = ring(ph, 'P', [128, 512], BF16, 3)
        osb = ring(ph, 'osb', [128, 4, 129], F32)
        den = ring(ph, 'den', [128, 4], F32)
        fac = ring(ph, 'fac', [128, 4], F32)
        oacc = ring(ph, 'oacc', [128, 4, 64], F32)
        imp = ring(ph, 'imp', [128, 64], F32)
        imp3 = ring(ph, 'imp3', [128, 64], F32)
        m8 = ring(ph, 'm8', [128, 16], F32)
        selb = ring(ph, 'selb', [128, 64], F32)
        nsb = ring(ph, 'nsb', [128, 64], BF16)
        NS = ring(ph, 'NS', [128, 4, 128], BF16)
        om = ring(ph, 'om', [128, 256], BF16)
        Sr = [ph.ps('S%d' % i, [128, 512], F32) for i in range(2)]
        Oa = [ph.ps('Oa%d' % i, [128, 129], F32) for i in range(4)]
        Tn = ph.ps('Tn', [128, 8, 128], BF16)
        for b in (QT, KsT, KwT, kcT, Ex, NS[0], NS[1]):
            kb.op('pool', [], [b], lambda en, b=b: en.memset(b[64:128], 0.0))
        kb.dma('sp', btm[:], d['c_btmask'][:, :, :], [], [btm], btm)
        kb.dma('sp', ovs[:], d['c_ov'][:, :, :], [], [ovs], ovs)
        for i in range(4):
            kb.dma('sp', exs[:], d['c_ex'][:, i * 1024:(i + 1) * 1024], [], [exs], exs)
            kb.op('dve', [exs], [Ex], lambda en, i=i: en.tensor_copy(out=Ex[0:64, i * 1024:(i + 1) * 1024], in_=exs[:]))
        kb.op('pool', [], [Vs], lambda en: en.memset(Vs[:], 1.0))
        kb.op('pool', [], [Vw], lambda en: en.memset(Vw[:], 1.0))
        kb.op('pool', [], [Vc], lambda en: en.memset(Vc[:], 1.0))
        kb.op('dve', [ovs], [Vc], lambda en: en.tensor_copy(out=Vc[:, :, 65:129], in_=ovs[:]))
        it = 0
        itq = 0
        for g in groups:
            kb.dma('sp', QT[0:64], d['nsa_qT'][:, g * 4:(g + 1) * 4, :], [d['nsa_qT_b']], [QT], QT)
            kb.dma('act', KsT[0:64], d['nsa_kT'][:, g, :], [d['nsa_kT_b']], [KsT], KsT)
            kb.dma('act', KwT[0:64], d['nsa_kT'][:, 2 + g, :], [d['nsa_kT_b']], [KwT], KwT)
            kb.dma('sp', kcT[0:64], d['nsa_kcT'][:, g, :], [d['nsa_kcT_b']], [kcT], kcT)
            kb.dma('sp', Vs[:, :, 0:64], d['nsa_v'][:, g, :].rearrange('(t p) e -> p t e', p=128), [d['nsa_v_b']], [Vs], Vs)
            kb.dma('act', Vw[:, :, 0:64], d['nsa_v'][:, 2 + g, :].rearrange('(t p) e -> p t e', p=128), [d['nsa_v_b']], [Vw], Vw)
            kb.dma('sp', Vc[:, :, 0:64], d['nsa_vc'][:, :, g, :], [d['nsa_vc_b']], [Vc], Vc)
            kb.dma('sp', bts[:], d['nsa_bt'][g], [], [bts], bts)
            kb.op('dve', [bts, btm], [Bt], lambda en: en.tensor_tensor(out=Bt[:].rearrange('p t (r q) -> p t r q', r=4), in0=bts[:],
                                                                      in1=btm[:].unsqueeze(2).broadcast_to([128, 4, 4, 128]), op=ALU.add))
            def prefetch(qt, r2):
                kb.dma('sp', cbs[r2][:], d['nsa_cbias'][qt, g], [], [cbs[r2]], cbs[r2])
                kb.dma('sp', cbm[r2][:], d['c_cmask'][qt], [], [cbm[r2]], cbm[r2])
                kb.dma('sp', frc[r2][:], d['c_force'][qt], [], [frc[r2]], frc[r2])
                kb.dma('sp', gt[r2][:], d['gates'][qt * 128:(qt + 1) * 128, :], [d['gates_b']], [gt[r2]], gt[r2])
                kb.op('dve', [cbs[r2], cbm[r2]], [CB[r2]], lambda en: en.tensor_tensor(
                    out=CB[r2][:].rearrange('p t (r q) -> p t r q', r=4), in0=cbs[r2][:], in1=cbm[r2][:].unsqueeze(2).broadcast_to([128, 2, 4, 128]), op=ALU.add))
            prefetch(qts[0], itq % 2)
            for qi, qt in enumerate(qts):
                r2 = itq % 2
                itq += 1
                qs = slice(qt * 128, (qt + 1) * 128)
                CB_ = CB[r2]
                rhsq = QT[:, :, qs]
                oacc_, osb_, den_, fac_, gt_ = oacc[r2], osb[r2], den[r2], fac[r2], gt[r2]

                def branch(tiles, width, bi):
                    nonlocal it
                    n = len(tiles)
                    slots = {}

                    def emit_s(ti):
                        nonlocal it
                        S_, P_ = Sr[it % 2], Pr[it % 3]
                        it += 1
                        slots[ti] = (S_, P_)
                        mms = tiles[ti][0]
                        for mi, (lh, rh, rd) in enumerate(mms):
                            kb.op('pe', rd, [S_], lambda en: en.matmul(S_[:, :], lhsT=lh, rhs=rh, start=(mi == 0), stop=(mi == len(mms) - 1)))
                    emit_s(0)
                    for ti, (mms, vrhs, vbuf) in enumerate(tiles):
                        if ti + 1 < n:
                            emit_s(ti + 1)
                        S_, P_ = slots[ti]
                        kb.op('act', [S_], [P_], lambda en: en.activation(out=P_[:], in_=S_[:, :], func=AF.Exp))
                        for r in range(4):
                            kb.op('pe', [P_, vbuf], [Oa[r]], lambda en, r=r: en.matmul(Oa[r][:, 0:width], lhsT=P_[:, r * 128:(r + 1) * 128], rhs=vrhs,
                                                                                      start=(ti == 0), stop=(ti == n - 1)))
                    for r in range(4):
                        kb.op('act', [Oa[r]], [osb_], lambda en, r=r: en.copy(out=osb_[:, r, 0:width], in_=Oa[r][:, 0:width]))
                    kb.op('dve', [osb_], [den_], lambda en: en.tensor_scalar(out=den_[:], in0=osb_[:, :, 64], scalar1=1e-30, scalar2=None, op0=ALU.max))
                    kb.op('dve', [den_], [den_], lambda en: en.reciprocal(out=den_[:], in_=den_[:]))
                    kb.op('dve', [den_, gt_], [fac_], lambda en: en.tensor_tensor(out=fac_[:], in0=den_[:], in1=gt_[:, g * 12 + bi:g * 12 + 12:3], op=ALU.mult))
                    if bi == 0:
                        kb.op('dve', [osb_, fac_], [oacc_], lambda en: en.tensor_tensor(out=oacc_[:], in0=osb_[:, :, 0:64], in1=bcast_mid(fac_[:], 64), op=ALU.mult))
                    else:
                        tmp = imp3[r2]
                        for r in range(4):
                            kb.op('dve', [osb_, fac_, oacc_], [oacc_], lambda en, r=r: en.scalar_tensor_tensor(
                                out=oacc_[:, r, :], in0=osb_[:, r, 0:64], scalar=fac_[:, r:r + 1], in1=oacc_[:, r, :], op0=ALU.mult, op1=ALU.add))

                nct = 1 if qt <= 15 else 2
                tiles = []
                for ct in range(nct):
                    tiles.append(([(kcT[:, ct * 128:(ct + 1) * 128], rhsq, [kcT, QT]), (ident[:], CB_[:, ct, :], [ident, CB_])], Vc[:, ct, :], Vc))
                branch(tiles, 129, 0)
                imp_, imp3_, m8_, selb_, nsb_, NS_ = imp[r2], imp3[r2], m8[r2], selb[r2], nsb[r2], NS[r2]
                kb.op('dve', [osb_, den_], [imp_], lambda en: en.tensor_scalar(out=imp_[:], in0=osb_[:, 0, 65:129], scalar1=den_[:, 0:1], scalar2=None, op0=ALU.mult))
                for r in range(1, 4):
                    kb.op('dve', [osb_, den_, imp_], [imp_], lambda en, r=r: en.scalar_tensor_tensor(
                        out=imp_[:], in0=osb_[:, r, 65:129], scalar=den_[:, r:r + 1], in1=imp_[:], op0=ALU.mult, op1=ALU.add))
                kb.op('dve', [imp_, frc[r2]], [imp_], lambda en: en.tensor_tensor(out=imp_[:], in0=imp_[:], in1=frc[r2][:], op=ALU.max))
                kb.op('dve', [imp_], [m8_], lambda en: en.max(out=m8_[:, 0:8], in_=imp_[:]))
                kb.op('dve', [imp_, m8_], [imp3_], lambda en: en.match_replace(out=imp3_[:], in_to_replace=m8_[:, 0:8], in_values=imp_[:], imm_value=-1.0))
                kb.op('dve', [imp3_], [m8_], lambda en: en.max(out=m8_[:, 8:16], in_=imp3_[:]))
                kb.op('dve', [imp_, m8_], [selb_], lambda en: en.tensor_scalar(out=selb_[:], in0=imp_[:], scalar1=m8_[:, 15:16], scalar2=None, op0=ALU.is_ge))
                kb.op('dve', [selb_], [nsb_], lambda en: en.tensor_scalar(out=nsb_[:], in0=selb_[:], scalar1=-1.0, scalar2=30000.0, op0=ALU.add, op1=ALU.mult))
                tiles = []
                for kt in range(max(0, qt - 4), qt + 1):
                    ty = 0 if kt == qt else (1 if kt == qt - 1 else (3 if kt == qt - 4 else 2))
                    ks = slice(kt * 128, (kt + 1) * 128)
                    tiles.append(([(KwT[:, ks], rhsq, [KwT, QT]), (ident[:], Bt[:, ty, :], [ident, Bt])], Vw[:, kt, :], Vw))
                branch(tiles, 65, 2)
                kb.op('pe', [nsb_, ident], [Tn], lambda en: en.transpose(out=Tn[0:64, 0, :], in_=nsb_[:], identity=ident[:]))
                for r in range(4):
                    kb.op('act', [Tn], [NS_], lambda en, r=r: en.copy(out=NS_[0:64, r, :], in_=Tn[0:64, 0, :]))
                if qi + 1 < len(qts):
                    prefetch(qts[qi + 1], itq % 2)
                tiles = []
                for kt in range(0, qt + 1):
                    ty = 0 if kt == qt else (1 if kt == qt - 1 else 2)
                    ks = slice(kt * 128, (kt + 1) * 128)
                    tiles.append(([(KsT[:, ks], rhsq, [KsT, QT]), (ident[:], Bt[:, ty, :], [ident, Bt]),
                                   (Ex[:, ks], NS_[:].rearrange('p r q -> p (r q)'), [Ex, NS_])], Vs[:, kt, :], Vs))
                branch(tiles, 65, 1)
                om_ = om[r2]
                kb.op('act', [oacc_], [om_], lambda en: en.copy(out=om_[:], in_=oacc_[:].rearrange('p r e -> p (r e)')))
                kb.dma('act', d['mix'][qs, g * 256:(g + 1) * 256], om_[:], [om_], [d['mix_b']], om_)


def build(phases=('e1',), debug_out=(), nt=NT, lim=99, ext_in=(), **kw):
    nc = bass.Bass("TRN2", target_bir_lowering=False)
    d = {}
    for name, shape in INPUTS.items():
        d[name] = nc.dram_tensor(name, shape, F32, kind="ExternalInput")
    for name, (shape, dt) in SCRATCH.items():
        kind = "ExternalOutput" if name in debug_out else ("ExternalInput" if name in ext_in else "Internal")
        d[name] = nc.dram_tensor(name, shape, dt, kind=kind)
        d[name + '_b'] = Buf(name, d[name], multi=True)
    d['out'] = nc.dram_tensor('out', [S, D], F32, kind="ExternalOutput")
    d['out_b'] = Buf('out', d['out'], multi=True)
    with ExitStack() as es:
        kb = KB(nc, es)
        if 'e1' in phases:
            phase_e1(kb, d, nt, lim)
        if 'mla' in phases:
            phase_mla(kb, d, kw.get('nqc', 8))
        if 'cmp' in phases:
            phase_cmp(kb, d)
        if 'nsa' in phases:
            phase_nsa(kb, d, kw.get('qts'), kw.get('groups', (0, 1)))
        if 'wout' in phases:
            phase_wout(kb, d, nt)
        if 'ffn' in phases:
            ps = []
            for i, (rsd, dst) in enumerate((('x1a', 'x1h'), ('x1h', 'x1'))):
                ps.append(dict(src='x1a', resid=rsd, dst=dst, grow=d['ev_ffn_row'], gate=None,
                               wg=d['ffn_w_gate'][:, i * DFE:(i + 1) * DFE], wu=d['ffn_w_up'][:, i * DFE:(i + 1) * DFE],
                               wd=d['ffn_w_down'][i * DFE:(i + 1) * DFE, :]))
            expert_passes(kb, d, ps, kw.get('nst', 8))
        if 'odd' in phases:
            phase_odd_mixer(kb, d, kw.get('nst', 8))
        if 'router' in phases:
            phase_router(kb, d, nt)
        if 'moe' in phases:
            ps = []
            ne = kw.get('ne', NE)
            chain = ['x2a'] + [('acc0', 'acc1')[i % 2] for i in range(ne - 1)] + ['out']
            for e in range(ne):
                ps.append(dict(src='x2a', resid=chain[e], dst=chain[e + 1], grow=d['od_ffn_row'], gate=('moe_gate', e),
                               wg=d['moe_w_gate'][e], wu=d['moe_w_up'][e], wd=d['moe_w_down'][e]))
            expert_passes(kb, d, ps, kw.get('nst', 8))
    return nc


ALL_PHASES = ('e1', 'cmp', 'nsa', 'mla', 'wout', 'ffn', 'odd', 'moe')
_NC_CACHE = {}


def kernel(**inputs):
    inp = {k: np.asarray(v) for k, v in inputs.items()}
    if 'nc' not in _NC_CACHE:
        _NC_CACHE['nc'] = build(ALL_PHASES)
    nc = _NC_CACHE['nc']
    consts = host_consts()
    n = inp['x'].shape[0]
    in_maps = [prep_core_inputs(inp, b, consts) for b in range(n)]
    res = run_bass_kernel_spmd(nc, in_maps, core_ids=list(range(n)))
    return np.stack([np.asarray(r['out'], dtype=np.float32) for r in res.results], axis=0)
```
